# Optimizing a Trainium2 kernel written in Bass

```python
import math
import jax, jax.numpy as jnp
from jax import lax
import numpy as np

D_MODEL = 1024
BATCH = 8
SEQ = 4096
DEPTH = 4

CTX_LEN = 256
GRID_W = 64

D_HYENA = 512
D_SCONV = 512
NA_HEADS = 8
NA_HEAD_DIM = 64
D_NA = NA_HEADS * NA_HEAD_DIM
N_BRANCH = 3
NA_WIN_ROWS = 8
NA_WIN_COLS = 16
NA_QBLK = 16
NA_KBLK = NA_QBLK + NA_WIN_COLS
HY_BANDS = 16
HY_EMB = 1 + 2 * HY_BANDS
HY_FILTER_DIM = 64
HY_FAST_DECAY = 0.3
HY_SLOW_DECAY = 1.5
HY_TARGET = 1e-2
N_EXPERTS = 64
N_GROUPS = 8
TOPK_GROUPS = 4
TOP_K = 8
D_EXPERT = 256
D_SHARED = 256
ROUTED_SCALE = 2.5
MOE_BLOCK = 256
LN_EPS = 1e-5
NEG_INF = -1e30

OFF_HY = 0
OFF_SC = OFF_HY + 3 * D_HYENA
OFF_NA = OFF_SC + 3 * D_SCONV
OFF_GATE = OFF_NA + 3 * D_NA
D_IN_PROJ = OFF_GATE + N_BRANCH * D_MODEL

kernel_name = 'hybrid_hyena_shortconv_natten_moe_trunk'


def _deepnorm_alpha():
    return (2.0 * DEPTH) ** 0.25


def _deepnorm_beta():
    return (8.0 * DEPTH) ** -0.25


def layer_norm(x, g, b):
    xf = x.astype(jnp.float32)
    mu = jnp.mean(xf, -1, keepdims=True)
    var = jnp.mean(jnp.square(xf - mu), -1, keepdims=True)
    y = (xf - mu) * lax.rsqrt(var + LN_EPS) * g.astype(jnp.float32) + b.astype(jnp.float32)
    return y.astype(x.dtype)


def dwconv3(u, w):
    up = jnp.pad(u, ((0, 0), (1, 1), (0, 0)))
    return up[:, :-2] * w[0] + up[:, 1:-1] * w[1] + up[:, 2:] * w[2]


def hyena_filter(L, w1, b1, w2, b2, w3, sin_freq):
    f32 = jnp.float32
    t = jnp.linspace(0.0, 1.0, L, dtype=f32)[:, None]
    ang = (2.0 * math.pi / L) * jnp.arange(L, dtype=f32)[:, None]
    bands = jnp.linspace(1e-4, HY_BANDS - 1, HY_BANDS, dtype=f32)[None, :]
    feats = jnp.concatenate([t, jnp.cos(bands * ang), -jnp.sin(bands * ang)], -1)
    z = jnp.sin(sin_freq[0].astype(f32) * (feats @ w1.astype(f32) + b1.astype(f32)))
    z = jnp.sin(sin_freq[1].astype(f32) * (z @ w2.astype(f32) + b2.astype(f32)))
    h = z @ w3.astype(f32)
    deltas = jnp.abs(jnp.linspace(math.log(HY_TARGET) / HY_SLOW_DECAY, math.log(HY_TARGET) / HY_FAST_DECAY, D_HYENA, dtype=f32))
    decay = jnp.exp(-t * deltas[None, :])
    h_fwd = h[:, :D_HYENA] * decay
    h_bwd = h[:, D_HYENA:] * decay
    return jnp.concatenate([h_fwd, jnp.zeros((1, D_HYENA), f32), h_bwd[:0:-1]], 0)


def hyena_mix(u3, conv_w, conv_b, w1, b1, w2, b2, w3, sin_freq, bias_d):
    L = u3.shape[1]
    uc = dwconv3(u3, conv_w) + conv_b
    x0, x1, v = jnp.split(uc, 3, axis=-1)
    z = v * x1
    g = hyena_filter(L, w1, b1, w2, b2, w3, sin_freq)
    n = 2 * L
    zf = jnp.fft.rfft(z.astype(jnp.float32), n=n, axis=1)
    y = jnp.fft.irfft(zf * jnp.fft.rfft(g, axis=0)[None], n=n, axis=1)[:, :L]
    return x0 * (y.astype(z.dtype) + z * bias_d)


def short_conv_mix(u3, conv_w):
    bg, cg, xs = jnp.split(u3, 3, axis=-1)
    return bg * dwconv3(cg * xs, conv_w)


def _heads(t):
    b, l, _ = t.shape
    return t.reshape(b, l, NA_HEADS, NA_HEAD_DIM).transpose(0, 2, 1, 3)


def context_attention(q, k, v):
    b, l, _ = q.shape
    qh, kh, vh = _heads(q), _heads(k), _heads(v)
    s = jnp.einsum('bhqd,bhkd->bhqk', qh, kh, preferred_element_type=jnp.float32) * (NA_HEAD_DIM ** -0.5)
    p = jax.nn.softmax(s, axis=-1).astype(vh.dtype)
    o = jnp.einsum('bhqk,bhkd->bhqd', p, vh)
    return o.transpose(0, 2, 1, 3).reshape(b, l, D_NA)


def _na_col_layout():
    wc = min(NA_WIN_COLS, GRID_W)
    qcol = np.arange(GRID_W)
    cstart = np.clip(qcol - wc // 2, 0, GRID_W - wc)
    ncb = GRID_W // NA_QBLK
    kbw = min(NA_KBLK, GRID_W)
    kb = np.minimum(cstart[::NA_QBLK], GRID_W - kbw)
    kcol = kb[:, None] + np.arange(kbw)[None, :]
    qc = qcol.reshape(ncb, NA_QBLK)
    cs = cstart.reshape(ncb, NA_QBLK)
    inwin = (kcol[:, None, :] >= cs[:, :, None]) & (kcol[:, None, :] < cs[:, :, None] + wc)
    rel = np.clip(kcol[:, None, :] - qc[:, :, None] + NA_WIN_COLS - 1, 0, 2 * NA_WIN_COLS - 2)
    return ncb, kbw, kcol, inwin, rel


def neighbourhood_attention(q, k, v, k_ctx, v_ctx, rpb):
    b, s, _ = q.shape
    rows = s // GRID_W
    wr = min(NA_WIN_ROWS, rows)
    ncb, kbw, kcol, inwin, rel = _na_col_layout()
    scale = NA_HEAD_DIM ** -0.5
    n_loc = wr * kbw

    def grid(t):
        return t.reshape(b, rows, GRID_W, NA_HEADS, NA_HEAD_DIM).transpose(1, 0, 3, 2, 4)

    qg = grid(q)
    kg = grid(k)[:, :, :, kcol]
    vg = grid(v)[:, :, :, kcol]
    kc, vc = _heads(k_ctx), _heads(v_ctx)
    bias_cols = rpb[:, :, rel]
    mask = jnp.asarray(np.broadcast_to(inwin[:, :, None, :], (ncb, NA_QBLK, wr, kbw)).reshape(ncb, NA_QBLK, n_loc))

    def row_step(r):
        rs = jnp.clip(r - wr // 2, 0, rows - wr)
        kr = lax.dynamic_slice_in_dim(kg, rs, wr, axis=0).transpose(1, 2, 3, 0, 4, 5).reshape(b, NA_HEADS, ncb, n_loc, NA_HEAD_DIM)
        vr = lax.dynamic_slice_in_dim(vg, rs, wr, axis=0).transpose(1, 2, 3, 0, 4, 5).reshape(b, NA_HEADS, ncb, n_loc, NA_HEAD_DIM)
        qr = qg[r].reshape(b, NA_HEADS, ncb, NA_QBLK, NA_HEAD_DIM)
        ridx = rs - r + jnp.arange(wr) + NA_WIN_ROWS - 1
        bias = bias_cols[:, ridx].transpose(0, 2, 3, 1, 4).reshape(NA_HEADS, ncb, NA_QBLK, n_loc)
        s_loc = jnp.einsum('bhnqd,bhnkd->bhnqk', qr, kr, preferred_element_type=jnp.float32) * scale + bias.astype(jnp.float32)
        s_loc = jnp.where(mask, s_loc, NEG_INF)
        s_ctx = jnp.einsum('bhnqd,bhcd->bhnqc', qr, kc, preferred_element_type=jnp.float32) * scale
        p = jax.nn.softmax(jnp.concatenate([s_loc, s_ctx], -1), axis=-1).astype(vr.dtype)
        o = jnp.einsum('bhnqk,bhnkd->bhnqd', p[..., :n_loc], vr) + jnp.einsum('bhnqc,bhcd->bhnqd', p[..., n_loc:], vc)
        return o.reshape(b, NA_HEADS, GRID_W, NA_HEAD_DIM)

    o = lax.map(row_step, jnp.arange(rows))
    return o.transpose(1, 0, 3, 2, 4).reshape(b, s, D_NA)


def mixer_output(p, y_attn, hy, sc_conv_w, hy_proj, sc_proj, na_proj, w_o):
    y_hy = hyena_mix(p[..., OFF_HY:OFF_SC], *hy)
    y_sc = short_conv_mix(p[..., OFF_SC:OFF_NA], sc_conv_w)
    g_hy, g_sc, g_na = jnp.split(jax.nn.sigmoid(p[..., OFF_GATE:]), 3, axis=-1)
    merged = g_hy * (y_hy @ hy_proj) + g_sc * (y_sc @ sc_proj) + g_na * (y_attn @ na_proj)
    return merged @ w_o


def moe_ffn(h, router_w, router_b, w1, w3, w2, s1, s3, s2):
    t_tok, d = h.shape
    scores = jax.nn.sigmoid((h @ router_w).astype(jnp.float32))
    sel = scores + router_b.astype(jnp.float32)
    per_group = N_EXPERTS // N_GROUPS
    group_score = lax.top_k(sel.reshape(t_tok, N_GROUPS, per_group), 2)[0].sum(-1)
    _, top_groups = lax.top_k(group_score, TOPK_GROUPS)
    group_mask = (top_groups[..., None] == jnp.arange(N_GROUPS)).any(axis=1)
    expert_mask = jnp.repeat(group_mask, per_group, axis=1)
    _, top_e = lax.top_k(jnp.where(expert_mask, sel, -jnp.inf), TOP_K)
    w = jnp.take_along_axis(scores, top_e, axis=1)
    w = w / jnp.sum(w, -1, keepdims=True) * ROUTED_SCALE
    tk = t_tok * TOP_K
    flat_e = top_e.reshape(tk)
    order = jnp.argsort(flat_e)
    e_s = flat_e[order]
    tok_s = (order // TOP_K).astype(jnp.int32)
    w_s = w.reshape(tk)[order]
    counts = jnp.bincount(flat_e, length=N_EXPERTS)
    padded = (counts + MOE_BLOCK - 1) // MOE_BLOCK * MOE_BLOCK
    start = jnp.cumsum(counts) - counts
    pstart = jnp.cumsum(padded) - padded
    dest = pstart[e_s] + jnp.arange(tk, dtype=jnp.int32) - start[e_s]
    n_blocks = -(-tk // MOE_BLOCK) + N_EXPERTS
    tok_buf = jnp.full((n_blocks * MOE_BLOCK,), t_tok, jnp.int32).at[dest].set(tok_s)
    w_buf = jnp.zeros((n_blocks * MOE_BLOCK,), h.dtype).at[dest].set(w_s.astype(h.dtype))
    blk_expert = jnp.minimum(jnp.searchsorted(jnp.cumsum(padded) // MOE_BLOCK, jnp.arange(n_blocks), side='right'), N_EXPERTS - 1)
    h_pad = jnp.concatenate([h, jnp.zeros((1, d), h.dtype)], 0)

    def block_step(acc, blk):
        tb, wb, e = blk
        xb = h_pad[tb]
        yb = (jax.nn.silu(xb @ w1[e]) * (xb @ w3[e])) @ w2[e]
        return acc.at[tb].add(yb * wb[:, None]), None

    routed, _ = lax.scan(block_step, jnp.zeros((t_tok + 1, d), h.dtype),
                         (tok_buf.reshape(n_blocks, MOE_BLOCK), w_buf.reshape(n_blocks, MOE_BLOCK), blk_expert))
    shared = (jax.nn.silu(h @ s1) * (h @ s3)) @ s2
    return routed[:t_tok] + shared


def setup_inputs(seed: int = 0) -> dict:
    key = jax.random.key(seed)
    ks = iter(jax.random.split(key, 40))
    beta = _deepnorm_beta()
    L = DEPTH

    def nrm(shape, scale):
        return jax.random.normal(next(ks), shape, jnp.float32) * scale

    return {
        'x': nrm((BATCH, SEQ, D_MODEL), 1.0),
        'c': nrm((BATCH, D_MODEL), 1.0),
        'ctx': nrm((BATCH, CTX_LEN, D_MODEL), 1.0),
        'c_ctx': nrm((D_MODEL,), 1.0),
        'w_ada': nrm((L, D_MODEL, 6 * D_MODEL), 0.5 * D_MODEL ** -0.5),
        'b_ada': nrm((L, 6 * D_MODEL), 0.01),
        'w_in': nrm((L, D_MODEL, D_IN_PROJ), D_MODEL ** -0.5),
        'hy_conv_w': nrm((L, 3, 3 * D_HYENA), 3 ** -0.5),
        'hy_conv_b': nrm((L, 3 * D_HYENA), 0.01),
        'hy_w1': nrm((L, HY_EMB, HY_FILTER_DIM), HY_EMB ** -0.5),
        'hy_b1': nrm((L, HY_FILTER_DIM), 0.01),
        'hy_w2': nrm((L, HY_FILTER_DIM, HY_FILTER_DIM), HY_FILTER_DIM ** -0.5),
        'hy_b2': nrm((L, HY_FILTER_DIM), 0.01),
        'hy_w3': nrm((L, HY_FILTER_DIM, 2 * D_HYENA), 0.03 * HY_FILTER_DIM ** -0.5),
        'hy_sin_freq': 1.0 + nrm((L, 2, HY_FILTER_DIM), 0.01),
        'hy_bias_d': nrm((L, D_HYENA), 0.1),
        'hy_proj': nrm((L, D_HYENA, D_MODEL), beta * D_HYENA ** -0.5),
        'sc_conv_w': nrm((L, 3, D_SCONV), 3 ** -0.5),
        'sc_proj': nrm((L, D_SCONV, D_MODEL), beta * D_SCONV ** -0.5),
        'na_rpb': nrm((L, NA_HEADS, 2 * NA_WIN_ROWS - 1, 2 * NA_WIN_COLS - 1), 0.02),
        'na_proj': nrm((L, D_NA, D_MODEL), beta * D_NA ** -0.5),
        'w_o': nrm((L, D_MODEL, D_MODEL), beta * D_MODEL ** -0.5),
        'ln1_g': 1.0 + nrm((L, D_MODEL), 0.01),
        'ln1_b': nrm((L, D_MODEL), 0.01),
        'ln2_g': 1.0 + nrm((L, D_MODEL), 0.01),
        'ln2_b': nrm((L, D_MODEL), 0.01),
        'moe_router': nrm((L, D_MODEL, N_EXPERTS), D_MODEL ** -0.5),
        'moe_bias': nrm((L, N_EXPERTS), 0.01),
        'moe_w1': nrm((L, N_EXPERTS, D_MODEL, D_EXPERT), D_MODEL ** -0.5),
        'moe_w3': nrm((L, N_EXPERTS, D_MODEL, D_EXPERT), D_MODEL ** -0.5),
        'moe_w2': nrm((L, N_EXPERTS, D_EXPERT, D_MODEL), beta * D_EXPERT ** -0.5),
        'sh_w1': nrm((L, D_MODEL, D_SHARED), D_MODEL ** -0.5),
        'sh_w3': nrm((L, D_MODEL, D_SHARED), D_MODEL ** -0.5),
        'sh_w2': nrm((L, D_SHARED, D_MODEL), beta * D_SHARED ** -0.5),
    }


def reference(x, c, ctx, c_ctx, w_ada, b_ada, w_in, hy_conv_w, hy_conv_b, hy_w1, hy_b1, hy_w2, hy_b2, hy_w3,
              hy_sin_freq, hy_bias_d, hy_proj, sc_conv_w, sc_proj, na_rpb, na_proj, w_o, ln1_g, ln1_b, ln2_g, ln2_b,
              moe_router, moe_bias, moe_w1, moe_w3, moe_w2, sh_w1, sh_w3, sh_w2):
    alpha = _deepnorm_alpha()
    b, s, d = x.shape
    n_lat = b * s
    cond = jax.nn.silu(c)
    cond_ctx = jax.nn.silu(c_ctx)
    xl, xc = x, ctx
    for i in range(DEPTH):
        last = i == DEPTH - 1
        mod_l = jnp.split((cond @ w_ada[i] + b_ada[i])[:, None, :], 6, axis=-1)
        mod_c = jnp.split(cond_ctx @ w_ada[i] + b_ada[i], 6, axis=-1)
        hy = (hy_conv_w[i], hy_conv_b[i], hy_w1[i], hy_b1[i], hy_w2[i], hy_b2[i], hy_w3[i], hy_sin_freq[i], hy_bias_d[i])

        hl = xl * (1 + mod_l[1]) + mod_l[0]
        hc = xc * (1 + mod_c[1]) + mod_c[0]
        pl = hl @ w_in[i]
        if last:
            k_c, v_c = jnp.split(hc @ w_in[i][:, OFF_NA + D_NA:OFF_GATE], 2, axis=-1)
        else:
            pc = hc @ w_in[i]
            q_c, k_c, v_c = jnp.split(pc[..., OFF_NA:OFF_GATE], 3, axis=-1)
        q_l, k_l, v_l = jnp.split(pl[..., OFF_NA:OFF_GATE], 3, axis=-1)
        att_l = neighbourhood_attention(q_l, k_l, v_l, k_c, v_c, na_rpb[i])
        out_l = mixer_output(pl, att_l, hy, sc_conv_w[i], hy_proj[i], sc_proj[i], na_proj[i], w_o[i])
        xl = layer_norm(alpha * xl + mod_l[2] * out_l, ln1_g[i], ln1_b[i])
        if not last:
            att_c = context_attention(q_c, k_c, v_c)
            out_c = mixer_output(pc, att_c, hy, sc_conv_w[i], hy_proj[i], sc_proj[i], na_proj[i], w_o[i])
            xc = layer_norm(alpha * xc + mod_c[2] * out_c, ln1_g[i], ln1_b[i])

        tokens = (xl * (1 + mod_l[4]) + mod_l[3]).reshape(n_lat, d)
        if not last:
            hc2 = xc * (1 + mod_c[4]) + mod_c[3]
            tokens = jnp.concatenate([tokens, hc2.reshape(-1, d)], 0)
        f = moe_ffn(tokens, moe_router[i], moe_bias[i], moe_w1[i], moe_w3[i], moe_w2[i], sh_w1[i], sh_w3[i], sh_w2[i])
        xl = layer_norm(alpha * xl + mod_l[5] * f[:n_lat].reshape(b, s, d), ln2_g[i], ln2_b[i])
        if not last:
            xc = layer_norm(alpha * xc + mod_c[5] * f[n_lat:].reshape(xc.shape), ln2_g[i], ln2_b[i])
    return xl
```

```python
import math
from contextlib import ExitStack
import numpy as np
import ml_dtypes
import concourse.bass as bass
import concourse.mybir as mybir
from concourse.bass_utils import run_bass_kernel_spmd

F32 = mybir.dt.float32
BF16 = mybir.dt.bfloat16
AF = mybir.ActivationFunctionType
ALU = mybir.AluOpType
AX = mybir.AxisListType

D = 1024
NLAT = 4096
NCTX = 256
T = NLAT + NCTX
DEPTH = 4
OFF_HY, OFF_SC, OFF_NA, OFF_GATE, DIN = 0, 1536, 3072, 4608, 7680
TT = [(i * 512, 512) for i in range(8)] + [(4096, 256)]
ALPHA = (2.0 * DEPTH) ** 0.25
LN_EPS = 1e-5
NPP = 160
MAGIC = 12582912.0
TWO_PI = 2.0 * math.pi


class Buf:
    __slots__ = ("name", "w", "r", "sem", "cnt")

    def __init__(self, name):
        self.name = name
        self.w = {}
        self.r = {}
        self.sem = None
        self.cnt = 0


class Ref:
    __slots__ = ("ap", "bufs", "dram")

    def __init__(self, ap, bufs, dram=False):
        self.ap = ap
        self.bufs = bufs
        self.dram = dram


class Tile:
    def __init__(self, handle, name, dram=False):
        self.h = handle
        self.buf = Buf(name)
        self.dram = dram

    def __getitem__(self, idx):
        return Ref(self.h[idx], [] if self.dram else [self.buf], self.dram)

    def ref(self, ap):
        return Ref(ap, [] if self.dram else [self.buf], self.dram)


class Prog:
    def __init__(self, nc, stack):
        self.nc = nc
        self.gstack = stack
        self.stack = stack
        self.eng = {"pe": nc.tensor, "act": nc.scalar, "dve": nc.vector, "pool": nc.gpsimd, "sp": nc.sync}
        self.sem = {}
        self.cnt = {}
        for e in ("pe", "act", "dve", "pool"):
            self.sem[e] = stack.enter_context(nc.semaphore("s_" + e))
            self.cnt[e] = 0
        self.waited = {e: {} for e in self.eng}
        self.sempool = []
        self.allsems = []
        self.phase_bufs = []
        self.ninst = 0
        self.nwait = 0
        self.uid = 0

    def sb(self, name, shape, dt):
        self.uid += 1
        h = self.stack.enter_context(self.nc.sbuf_tensor("%s_%d" % (name, self.uid), list(shape), dt))
        t = Tile(h, name)
        self.phase_bufs.append(t.buf)
        return t

    def ps(self, name, shape, dt=F32):
        h = self.gstack.enter_context(self.nc.psum_tensor(name, list(shape), dt))
        return Tile(h, name)

    def dram(self, name, shape, dt, kind="Internal"):
        h = self.nc.dram_tensor(name, list(shape), dt, kind=kind)
        return Tile(h, name, dram=True)

    def _dsem(self, buf):
        if buf.sem is None:
            if self.sempool:
                ent = self.sempool.pop()
            else:
                ent = [self.gstack.enter_context(self.nc.semaphore("d%d" % len(self.allsems))), 0]
                self.allsems.append(ent)
            buf.sem = ent
            buf.cnt = ent[1]
        return buf.sem[0]

    class _Phase:
        def __init__(self, P):
            self.P = P

        def __enter__(self):
            P = self.P
            self.prev_stack = P.stack
            self.prev_bufs = P.phase_bufs
            self.es = ExitStack()
            self.es.__enter__()
            P.stack = self.es
            P.phase_bufs = []
            return self

        def __exit__(self, *a):
            P = self.P
            P.barrier()
            for b in P.phase_bufs:
                if b.sem is not None:
                    b.sem[1] = b.cnt
                    P.sempool.append(b.sem)
                    b.sem = None
                if b in P._live_set:
                    P._live_set.discard(b)
            P._live = [b for b in P._live if b in P._live_set]
            P.stack = self.prev_stack
            P.phase_bufs = self.prev_bufs
            self.es.__exit__(None, None, None)
            return False

    def phase(self):
        return Prog._Phase(self)

    def barrier(self):
        toks = [(self.sem[e], self.cnt[e]) for e in self.sem if self.cnt[e] > 0]
        live = {}
        for b in self._all_live_bufs():
            if b.sem is not None and b.cnt > 0:
                live[id(b.sem[0])] = (b.sem[0], b.cnt)
        toks += list(live.values())
        for e, en in self.eng.items():
            wd = self.waited[e]
            for s, v in toks:
                if wd.get(id(s), 0) < v:
                    en.wait_ge(s, v)
                    wd[id(s)] = v
                    self.nwait += 1

    def _all_live_bufs(self):
        return self._live

    def _deps(self, e, reads, writes):
        deps = {}
        for b in reads:
            for k, (s, v) in b.w.items():
                if deps.get(k, (None, 0))[1] < v:
                    deps[k] = (s, v)
        for b in writes:
            for d in (b.w, b.r):
                for k, (s, v) in d.items():
                    if deps.get(k, (None, 0))[1] < v:
                        deps[k] = (s, v)
        wd = self.waited[e]
        en = self.eng[e]
        own = id(self.sem["pe"]) if e == "pe" else None
        for k, (s, v) in deps.items():
            if k == own:
                continue
            if wd.get(k, 0) < v:
                en.wait_ge(s, v)
                wd[k] = v
                self.nwait += 1

    def _done(self, tok, reads, writes):
        k = id(tok[0])
        for b in writes:
            b.w = {k: tok}
            b.r = {}
        for b in reads:
            if b.r.get(k, (None, 0))[1] < tok[1]:
                b.r[k] = tok

    def op(self, e, fn, reads, writes):
        rb = [b for r in reads for b in r.bufs]
        wb = [b for r in writes for b in r.bufs]
        self._deps(e, rb, wb)
        ins = fn(self.eng[e])
        self.cnt[e] += 1
        ins.then_inc(self.sem[e], 1)
        self.ninst += 1
        self._done((self.sem[e], self.cnt[e]), rb, wb)
        return ins

    def mmx(self, steps, writes):
        rb = [b for st in steps for x in st[1:3] for b in x.bufs]
        wb = [b for r in writes for b in r.bufs]
        self._deps("pe", rb, wb)
        ins = None
        en = self.eng["pe"]
        for (o, l, r, s0, s1) in steps:
            ins = en.matmul(o, l.ap, r.ap, start=s0, stop=s1)
            self.ninst += 1
        self.cnt["pe"] += 1
        ins.then_inc(self.sem["pe"], 1)
        self._done((self.sem["pe"], self.cnt["pe"]), rb, wb)

    def mm(self, out, pairs):
        n = len(pairs)
        self.mmx([(out.ap, l, r, i == 0, i == n - 1) for i, (l, r) in enumerate(pairs)], [out])

    def dma(self, q, out, in_, **kw):
        if out.dram and q == "sp":
            q = "pool"
        rb = list(in_.bufs)
        wb = list(out.bufs)
        self._deps(q, rb, wb)
        side = out.bufs[0] if not out.dram else in_.bufs[0]
        s = self._dsem(side)
        if side not in self._live_set:
            self._live_set.add(side)
            self._live.append(side)
        ins = self.eng[q].dma_start(out=out.ap, in_=in_.ap, **kw)
        side.cnt += 16
        ins.then_inc(s, 16)
        self.ninst += 1
        self._done((s, side.cnt), rb, wb)

    def act(self, out, in_, func, bias=None, scale=1.0):
        reads = [in_]
        kw = {}
        if isinstance(bias, Ref):
            reads.append(bias)
            kw["bias"] = bias.ap
        elif bias is not None:
            kw["bias"] = bias
        if isinstance(scale, Ref):
            reads.append(scale)
            kw["scale"] = scale.ap
        else:
            kw["scale"] = scale
        return self.op("act", lambda en: en.activation(out.ap, in_.ap, func, **kw), reads, [out])

    def amul(self, out, in_, m):
        reads = [in_]
        v = m
        if isinstance(m, Ref):
            reads.append(m)
            v = m.ap
        return self.op("act", lambda en: en.mul(out.ap, in_.ap, v), reads, [out])

    def tt(self, out, a, b, op, e="dve"):
        return self.op(e, lambda en: en.tensor_tensor(out.ap, a.ap, b.ap, op), [a, b], [out])

    def ts(self, out, a, s1, op0, s2=None, op1=None, e="dve"):
        reads = [a]
        v1 = s1
        if isinstance(s1, Ref):
            reads.append(s1)
            v1 = s1.ap
        v2 = s2
        if isinstance(s2, Ref):
            reads.append(s2)
            v2 = s2.ap
        kw = {}
        if op1 is not None:
            kw["op1"] = op1
        return self.op(e, lambda en: en.tensor_scalar(out.ap, a.ap, v1, v2, op0, **kw), reads, [out])

    def stt(self, out, a, s, b, op0, op1, e="dve"):
        e = "dve"
        reads = [a, b]
        v = s
        if isinstance(s, Ref):
            reads.append(s)
            v = s.ap
        return self.op(e, lambda en: en.scalar_tensor_tensor(out.ap, a.ap, v, b.ap, op0, op1), reads, [out])

    def copy(self, out, in_, e="dve"):
        if e == "act":
            return self.op("act", lambda en: en.copy(out.ap, in_.ap), [in_], [out])
        return self.op(e, lambda en: en.tensor_copy(out.ap, in_.ap), [in_], [out])

    def memset(self, out, val, e="pool"):
        return self.op(e, lambda en: en.memset(out.ap, val), [], [out])

    def recip(self, out, in_):
        return self.op("dve", lambda en: en.reciprocal(out.ap, in_.ap), [in_], [out])

    def rmax(self, out, in_):
        return self.op("dve", lambda en: en.tensor_reduce(out.ap, in_.ap, AX.X, ALU.max), [in_], [out])

    def rsum(self, out, in_):
        return self.op("dve", lambda en: en.tensor_reduce(out.ap, in_.ap, AX.X, ALU.add), [in_], [out])

    def top8(self, out, in_):
        return self.op("dve", lambda en: en.max(out.ap, in_.ap), [in_], [out])


def build(nlay=DEPTH, dbg=()):
    nc = bass.Bass("TRN2", target_bir_lowering=False)
    st = ExitStack()
    with st:
        P = Prog(nc, st)
        P._live = []
        P._live_set = set()
        L = nlay
        ext = lambda n, s, dt=F32: P.dram(n, s, dt, kind="ExternalInput")
        xT_in = ext("xT", [D, T])
        cvec = ext("cvec", [128, 8, 2])
        w_ada = ext("w_ada", [L, D, 6 * D])
        pp_d = ext("pp", [L, 128, NPP])
        w_in = ext("w_in", [L, D, DIN])
        hy_w1 = ext("hy_w1", [L, 33, 64])
        hy_w2 = ext("hy_w2", [L, 64, 64])
        hy_w3 = ext("hy_w3", [L, 64, 1024])
        projs = [ext(n, [L, 512, D]) for n in ("hy_proj", "sc_proj", "na_proj")]
        w_o = ext("w_o", [L, D, D])
        rpbx = ext("rpbx", [L, 64, 8 * 15 * 64])
        moe_router = ext("moe_router", [L, D, 64])
        moe_biasb = ext("moe_biasb", [L, 128, 64])
        moe_w1 = ext("moe_w1", [L, 64, D, 256])
        moe_w3 = ext("moe_w3", [L, 64, D, 256])
        moe_w2 = ext("moe_w2", [L, 64, 256, D])
        sh_w1 = ext("sh_w1", [L, D, 256])
        sh_w3 = ext("sh_w3", [L, D, 256])
        sh_w2 = ext("sh_w2", [L, 256, D])
        Fc = ext("Fc", [32, 128, 32 * 128], BF16)
        Fs = ext("Fs", [32, 128, 32 * 128], BF16)
        FcT = ext("FcT", [16, 128, 32 * 256], BF16)
        FsT = ext("FsT", [16, 128, 32 * 256], BF16)
        cFc = ext("cFc", [2, 128, 2 * 128], BF16)
        cFs = ext("cFs", [2, 128, 2 * 128], BF16)
        cFcT = ext("cFcT", [1, 128, 2 * 256], BF16)
        cFsT = ext("cFsT", [1, 128, 2 * 256], BF16)
        featsT = ext("featsT", [33, NLAT])
        cfeatsT = ext("cfeatsT", [33, NCTX])
        decay = ext("decay", [NLAT, 512])
        cdecay = ext("cdecay", [NCTX, 512])
        ident_d = ext("ident", [128, 128])
        yT = P.dram("yT", [D, NLAT], F32, kind="ExternalOutput")
        dbg_out = {}

        XT = P.dram("XT", [D, T], F32)
        PD = P.dram("PD", [DIN, T], F32)
        VT = P.dram("VT", [T, 512], BF16)
        ZD = P.dram("ZD", [512, T], F32)
        X0D = P.dram("X0D", [512, T], F32)
        ZT = P.dram("ZT", [T, 512], BF16)
        YB = [P.dram(n, [512, T], BF16) for n in ("YHY", "YSC", "YNA")]
        HTD = P.dram("HTD", [NLAT, 1024], BF16)
        cHTD = P.dram("cHTD", [NCTX, 1024], BF16)
        GRE = P.dram("GRE", [NLAT, 512], F32)
        GIM = P.dram("GIM", [NLAT, 512], F32)
        cGRE = P.dram("cGRE", [NCTX, 512], F32)
        cGIM = P.dram("cGIM", [NCTX, 512], F32)
        WG = P.dram("WG", [64, T], F32)

        ps = [P.ps("ps%d" % i, [128, 512]) for i in range(8)]

        ppall = P.sb("ppall", [128, L, NPP], F32)
        mods = P.sb("mods", [128, L, 48, 2], F32)
        identf = P.sb("identf", [128, 128], F32)
        identb = P.sb("identb", [128, 128], BF16)
        onesf = P.sb("onesf", [128, 128], F32)
        onesb = P.sb("onesb", [128, 64], BF16)
        onesb2 = P.sb("onesb2", [128, 128], BF16)
        biasb = P.sb("biasb", [128, L, 64], F32)

        def ppc(l, c, n=1, p0=0, p1=128):
            return Ref(ppall.h[p0:p1, l, c:c + n], [ppall.buf])

        def mod(l, which, kc, kind):
            return Ref(mods.h[:, l, which * 8 + kc, kind:kind + 1], [mods.buf])

        def sub(t, ap):
            return Ref(ap, [t.buf])

        with P.phase():
            for l in range(L):
                P.dma("sp", Ref(ppall.h[:, l, :], [ppall.buf]), pp_d[l])
                P.dma("sp", Ref(biasb.h[:, l, :], [biasb.buf]), moe_biasb[l])
            P.dma("sp", identf[:], ident_d[:])
            P.copy(identb[:], identf[:])
            P.memset(onesf[:], 1.0)
            P.memset(onesb[:], 1.0)
            P.memset(onesb2[:], 1.0)
            cv = P.sb("cv", [128, 8, 2], F32)
            P.dma("sp", cv[:], cvec[:])
            cond = P.sb("cond", [128, 8, 2], F32)
            P.act(cond[:], cv[:], AF.Silu)
            was = [P.sb("wa%d" % i, [128, 8, 1024], F32) for i in range(2)]
            i = 0
            for l in range(L):
                for mb in range(6):
                    wa = was[i % 2]
                    for kc in range(8):
                        P.dma("sp", sub(wa, wa.h[:, kc, :]), w_ada.ref(w_ada.h[l, kc * 128:(kc + 1) * 128, mb * 1024:(mb + 1) * 1024]))
                    for mi in range(8):
                        m = mb * 8 + mi
                        pb = ps[mi % 4]
                        o = sub(pb, pb.h[:, 0:2])
                        P.mm(o, [(sub(wa, wa.h[:, kc, mi * 128:(mi + 1) * 128]), sub(cond, cond.h[:, kc, :])) for kc in range(8)])
                        P.ts(sub(mods, mods.h[:, l, m, :]), o, ppc(l, m), ALU.add)
                    i += 1
                for w0 in (8, 32):
                    r = sub(mods, mods.h[:, l, w0:w0 + 8, :])
                    P.ts(r, r, 1.0, ALU.add)
            xs = [P.sb("xinit%d" % i, [128, T], F32) for i in range(2)]
            for kc in range(8):
                x_ = xs[kc % 2]
                P.dma("sp", x_[:], xT_in.ref(xT_in.h[kc * 128:(kc + 1) * 128, :]))
                P.dma("sp", XT.ref(XT.h[kc * 128:(kc + 1) * 128, :]), x_[:])

        def layer_norm(y, n, l, gc, bc, work):
            sq, mean, rstd = work
            yv = sub(y, y.h[:, :, :n])
            P.act(sub(sq, sq.h[:, :, :n]), yv, AF.Square)
            s1 = sub(ps[0], ps[0].h[:, :n])
            s2 = sub(ps[1], ps[1].h[:, :n])
            P.mm(s1, [(onesf[:], sub(y, y.h[:, m, :n])) for m in range(8)])
            P.mm(s2, [(onesf[:], sub(sq, sq.h[:, m, :n])) for m in range(8)])
            mn = sub(mean, mean.h[:, :n])
            rs = sub(rstd, rstd.h[:, :n])
            P.amul(mn, s1, 1.0 / D)
            P.tt(rs, mn, mn, ALU.mult)
            P.stt(rs, s2, 1.0 / D, rs, ALU.mult, ALU.subtract)
            P.ts(rs, rs, LN_EPS, ALU.add)
            P.act(rs, rs, AF.Sqrt)
            P.recip(rs, rs)
            bc3 = lambda t_, ap2: Ref(ap2.unsqueeze(1).broadcast_to([128, 8, n]), [t_.buf])
            P.tt(yv, yv, bc3(mean, mean.h[:, :n]), ALU.subtract)
            P.tt(yv, yv, bc3(rstd, rstd.h[:, :n]), ALU.mult)
            P.tt(yv, yv, Ref(ppall.h[:, l, gc:gc + 8].unsqueeze(2).broadcast_to([128, 8, n]), [ppall.buf]), ALU.mult)
            P.tt(yv, yv, Ref(ppall.h[:, l, bc:bc + 8].unsqueeze(2).broadcast_to([128, 8, n]), [ppall.buf]), ALU.add)

        def sin_rr(a, tmp):
            P.ts(tmp, a, 1.0 / TWO_PI, ALU.mult, MAGIC, ALU.add)
            P.ts(tmp, tmp, -MAGIC, ALU.add)
            P.stt(a, tmp, -TWO_PI, a, ALU.mult, ALU.add)
            P.act(a, a, AF.Sin)

        cast_rr = [0]

        def cast(out, in_):
            e = ("dve", "act", "dve", "act", "pool", "act", "dve", "act")[cast_rr[0] % 8]
            cast_rr[0] += 1
            P.copy(out, in_, e=e)

        for l in range(L):
            last = (l == L - 1)
            with P.phase():
                win = P.sb("win", [128, 8, DIN], BF16)
                wst = [P.sb("wst%d" % i, [128, 1920], F32) for i in range(2)]
                i = 0
                for kc in range(8):
                    for q in range(4):
                        s_ = wst[i % 2]
                        P.dma("sp", s_[:], w_in.ref(w_in.h[l, kc * 128:(kc + 1) * 128, q * 1920:(q + 1) * 1920]))
                        cast(sub(win, win.h[:, kc, q * 1920:(q + 1) * 1920]), s_[:])
                        i += 1
                xts = [P.sb("xt%d" % i, [128, 8, 512], F32) for i in range(1)]
                hls = [P.sb("hl%d" % i, [128, 8, 512], BF16) for i in range(2)]
                stg = [P.sb("stg%d" % i, [128, 512], F32) for i in range(4)]
                vst = [P.sb("vst%d" % i, [128, 512], BF16) for i in range(2)]
                si = 0
                for ti, (t0, n) in enumerate(TT):
                    kind = 0 if ti < 8 else 1
                    xt = xts[0]
                    hl = hls[ti % 2]
                    for kc in range(8):
                        P.dma("sp", sub(xt, xt.h[:, kc, :n]), XT.ref(XT.h[kc * 128:(kc + 1) * 128, t0:t0 + n]))
                    for kc in range(8):
                        P.act(sub(hl, hl.h[:, kc, :n]), sub(xt, xt.h[:, kc, :n]), AF.Identity,
                              bias=mod(l, 0, kc, kind), scale=mod(l, 1, kc, kind))
                    for j in range(DIN // 128):
                        if 32 <= j < 36:
                            continue
                        pb = ps[j % 4]
                        o = sub(pb, pb.h[:, :n])
                        P.mm(o, [(sub(win, win.h[:, kc, j * 128:(j + 1) * 128]), sub(hl, hl.h[:, kc, :n])) for kc in range(8)])
                        s_ = stg[si % 4]
                        so = sub(s_, s_.h[:, :n])
                        if si % 2 == 0:
                            P.copy(so, o, e="dve")
                        else:
                            P.copy(so, o, e="act")
                        P.dma("sp", PD.ref(PD.h[j * 128:(j + 1) * 128, t0:t0 + n]), so)
                        si += 1
                    for s in range(n // 128):
                        pb = ps[4 + s % 2]
                        P.mm(pb[:], [(sub(hl, hl.h[:, kc, s * 128:(s + 1) * 128]), sub(win, win.h[:, kc, OFF_NA + 1024:OFF_NA + 1536])) for kc in range(8)])
                        v_ = vst[s % 2]
                        P.copy(v_[:], pb[:], e="dve" if s % 2 else "act")
                        P.dma("sp", VT.ref(VT.h[t0 + s * 128:t0 + (s + 1) * 128, :]), v_[:])

            with P.phase():
                uu2 = [[P.sb("u%d%d" % (k, i), [128, T], F32) for i in range(3)] for k in range(2)]
                o3 = [P.sb("o%d" % i, [128, T], F32) for i in range(3)]
                zb = P.sb("zb", [128, T], BF16)
                zts = [P.sb("zts%d" % i, [128, 4, 128], BF16) for i in range(2)]

                def dwconv(o, uu, wcol, bcol, eng):
                    if bcol is None:
                        P.amul(o[:], uu[:], ppc(l, wcol + 1))
                    else:
                        P.act(o[:], uu[:], AF.Identity, bias=ppc(l, bcol), scale=ppc(l, wcol + 1))
                    for (a, Lc) in ((0, NLAT), (NLAT, NCTX)):
                        P.stt(sub(o, o.h[:, a + 1:a + Lc]), sub(uu, uu.h[:, a:a + Lc - 1]), ppc(l, wcol), sub(o, o.h[:, a + 1:a + Lc]), ALU.mult, ALU.add, e=eng)
                        P.stt(sub(o, o.h[:, a:a + Lc - 1]), sub(uu, uu.h[:, a + 1:a + Lc]), ppc(l, wcol + 2), sub(o, o.h[:, a:a + Lc - 1]), ALU.mult, ALU.add, e=eng)

                for j in range(4):
                    u = uu2[j % 2]
                    for q in range(3):
                        r0 = OFF_HY + q * 512 + j * 128
                        P.dma("sp", u[q][:], PD.ref(PD.h[r0:r0 + 128, :]))
                    for q in range(3):
                        c12 = q * 4 + j
                        dwconv(o3[q], u[q], 48 + c12 * 3, 84 + c12, "pool" if q == 0 else "dve")
                    P.tt(u[2][:], o3[2][:], o3[1][:], ALU.mult)
                    P.dma("sp", ZD.ref(ZD.h[j * 128:(j + 1) * 128, :]), u[2][:])
                    P.dma("sp", X0D.ref(X0D.h[j * 128:(j + 1) * 128, :]), o3[0][:])
                    P.copy(zb[:], u[2][:], e="act")
                    nblk = T // 128
                    g0 = 0
                    gi = 0
                    while g0 < nblk:
                        g = min(4, nblk - g0)
                        pb = ps[gi % 2]
                        for b in range(g):
                            P.mm(sub(pb, pb.h[:, b * 128:(b + 1) * 128]), [(sub(zb, zb.h[:, (g0 + b) * 128:(g0 + b + 1) * 128]), identb[:])])
                        z_ = zts[gi % 2]
                        P.copy(sub(z_, z_.h[:, 0:g, :]), sub(pb, pb.h[:, 0:g * 128].rearrange("p (b c) -> p b c", b=g)), e="dve" if gi % 2 else "act")
                        P.dma("sp", ZT.ref(ZT.h[g0 * 128:(g0 + g) * 128, j * 128:(j + 1) * 128].rearrange("(b p) c -> p b c", p=128)),
                              sub(z_, z_.h[:, 0:g, :]))
                        g0 += g
                        gi += 1
                for j in range(4):
                    u = uu2[j % 2]
                    for q in range(3):
                        r0 = OFF_SC + q * 512 + j * 128
                        P.dma("sp", u[q][:], PD.ref(PD.h[r0:r0 + 128, :]))
                    P.tt(u[1][:], u[1][:], u[2][:], ALU.mult, e="pool")
                    dwconv(o3[1], u[1], 100 + j * 3, None, "dve")
                    P.tt(zb[:], u[0][:], o3[1][:], ALU.mult)
                    P.dma("sp", YB[1].ref(YB[1].h[j * 128:(j + 1) * 128, :]), zb[:])

            for (Lc, fe_d, de_d, HT_d, Fc_d, Fs_d, GRE_d, GIM_d) in ((NLAT, featsT, decay, HTD, Fc, Fs, GRE, GIM),
                                                                      (NCTX, cfeatsT, cdecay, cHTD, cFc, cFs, cGRE, cGIM)):
                nb = Lc // 128
                with P.phase():
                    w1s = P.sb("w1s", [33, 64], F32)
                    w2s = P.sb("w2s", [64, 64], F32)
                    w3s = P.sb("w3s", [64, 1024], F32)
                    P.dma("sp", w1s[:], hy_w1[l])
                    P.dma("sp", w2s[:], hy_w2[l])
                    P.dma("sp", w3s[:], hy_w3[l])
                    b1, b2, f1, f2 = (ppc(l, 144 + i, 1, 0, 64) for i in range(4))
                    ft = P.sb("ft", [33, 512], F32)
                    a1 = P.sb("a1", [64, 512], F32)
                    a2 = P.sb("a2", [64, 512], F32)
                    tmp = P.sb("tmp", [64, 512], F32)
                    dec = [P.sb("dec%d" % i, [128, 512], F32) for i in range(2)]
                    hb = [P.sb("hb%d" % i, [128, 1024], BF16) for i in range(2)]
                    hfb = [P.sb("hfb%d" % i, [128, 512], F32) for i in range(2)]
                    n = min(512, Lc)
                    di = 0
                    for ch in range(max(1, Lc // 512)):
                        P.dma("sp", sub(ft, ft.h[:, :n]), fe_d.ref(fe_d.h[:, ch * 512:ch * 512 + n]))
                        p0 = sub(ps[0], ps[0].h[0:64, :n])
                        P.mm(p0, [(w1s[:], sub(ft, ft.h[:, :n]))])
                        a1r = sub(a1, a1.h[:, :n])
                        tr = sub(tmp, tmp.h[:, :n])
                        P.ts(a1r, p0, b1, ALU.add, f1, ALU.mult)
                        sin_rr(a1r, tr)
                        p1 = sub(ps[1], ps[1].h[0:64, :n])
                        P.mm(p1, [(w2s[:], a1r)])
                        a2r = sub(a2, a2.h[:, :n])
                        P.ts(a2r, p1, b2, ALU.add, f2, ALU.mult)
                        sin_rr(a2r, tr)
                        for s in range(n // 128):
                            row0 = ch * 512 + s * 128
                            h_ = hb[s % 2]
                            d_ = dec[di % 2]
                            di += 1
                            P.dma("sp", d_[:], de_d.ref(de_d.h[row0:row0 + 128, :]))
                            for half in range(2):
                                pb = ps[2 + half]
                                P.mm(pb[:], [(sub(a2, a2.h[:, s * 128:(s + 1) * 128]), sub(w3s, w3s.h[:, half * 512:(half + 1) * 512]))])
                                P.tt(hfb[half][:], pb[:], d_[:], ALU.mult)
                            if row0 == 0:
                                P.memset(sub(hfb[1], hfb[1].h[0:1, :]), 0.0, e="dve")
                            P.tt(sub(h_, h_.h[:, 0:512]), hfb[0][:], hfb[1][:], ALU.add)
                            P.tt(sub(h_, h_.h[:, 512:1024]), hfb[1][:], hfb[0][:], ALU.subtract)
                            P.dma("sp", HT_d.ref(HT_d.h[row0:row0 + 128, :]), h_[:])
                with P.phase():
                    hts = P.sb("hts", [128, nb, 1024], BF16)
                    for b0 in range(0, nb, 8):
                        b1_ = min(nb, b0 + 8)
                        P.dma("sp", sub(hts, hts.h[:, b0:b1_, :]), HT_d.ref(HT_d.h[b0 * 128:b1_ * 128, :].rearrange("(b p) c -> p b c", p=128)))
                    fcs = [P.sb("fc%d" % i, [128, nb, 128], BF16) for i in range(4)]
                    fss = [P.sb("fs%d" % i, [128, nb, 128], BF16) for i in range(4)]
                    tc_ = P.sb("tc", [128, 512], F32)
                    ts_ = P.sb("ts", [128, 512], F32)
                    gre = [P.sb("gre%d" % i, [128, 512], F32) for i in range(2)]
                    gim = [P.sb("gim%d" % i, [128, 512], F32) for i in range(2)]
                    for kt in range(nb):
                        fc = fcs[kt % 4]
                        fs = fss[kt % 4]
                        P.dma("sp", fc[:], Fc_d.ref(Fc_d.h[kt].rearrange("p (b k) -> p b k", b=nb)))
                        P.dma("sp", fs[:], Fs_d.ref(Fs_d.h[kt].rearrange("p (b k) -> p b k", b=nb)))
                        pc_ = ps[(kt % 2) * 2]
                        ps_ = ps[(kt % 2) * 2 + 1]
                        P.mm(pc_[:], [(sub(fc, fc.h[:, b, :]), sub(hts, hts.h[:, b, 0:512])) for b in range(nb)])
                        P.mm(ps_[:], [(sub(fs, fs.h[:, b, :]), sub(hts, hts.h[:, b, 512:1024])) for b in range(nb)])
                        P.copy(gre[kt % 2][:], pc_[:], e="act")
                        P.copy(gim[kt % 2][:], ps_[:], e="dve")
                        P.dma("sp", GRE_d.ref(GRE_d.h[kt * 128:(kt + 1) * 128, :]), gre[kt % 2][:])
                        P.dma("sp", GIM_d.ref(GIM_d.h[kt * 128:(kt + 1) * 128, :]), gim[kt % 2][:])

            for (Lc, tok0, Fc_d, Fs_d, FcT_d, FsT_d, GRE_d, GIM_d) in ((NLAT, 0, Fc, Fs, FcT, FsT, GRE, GIM),
                                                                        (NCTX, NLAT, cFc, cFs, cFcT, cFsT, cGRE, cGIM)):
                nb = Lc // 128
                with P.phase():
                    yre = P.sb("yre", [128, nb, 512], BF16)
                    yim = P.sb("yim", [128, nb, 512], BF16)
                    with P.phase():
                        zt = P.sb("zt", [128, nb, 512], BF16)
                        for b0 in range(0, nb, 8):
                            b1_ = min(nb, b0 + 8)
                            P.dma("sp", sub(zt, zt.h[:, b0:b1_, :]), ZT.ref(ZT.h[tok0 + b0 * 128:tok0 + b1_ * 128, :].rearrange("(b p) c -> p b c", p=128)))
                        fcs = [P.sb("fc%d" % i, [128, nb, 128], BF16) for i in range(4)]
                        fss = [P.sb("fs%d" % i, [128, nb, 128], BF16) for i in range(4)]
                        gre = [P.sb("gre%d" % i, [128, 512], F32) for i in range(2)]
                        gim = [P.sb("gim%d" % i, [128, 512], F32) for i in range(2)]
                        t1 = P.sb("t1", [128, 512], F32)
                        t2 = P.sb("t2", [128, 512], F32)
                        t3 = P.sb("t3", [128, 512], F32)
                        t4 = P.sb("t4", [128, 512], F32)
                        for kt in range(nb):
                            fc = fcs[kt % 4]
                            fs = fss[kt % 4]
                            P.dma("sp", fc[:], Fc_d.ref(Fc_d.h[kt].rearrange("p (b k) -> p b k", b=nb)))
                            P.dma("sp", fs[:], Fs_d.ref(Fs_d.h[kt].rearrange("p (b k) -> p b k", b=nb)))
                            P.dma("sp", gre[kt % 2][:], GRE_d.ref(GRE_d.h[kt * 128:(kt + 1) * 128, :]))
                            P.dma("sp", gim[kt % 2][:], GIM_d.ref(GIM_d.h[kt * 128:(kt + 1) * 128, :]))
                            pc = ps[(kt % 2) * 2]
                            pS = ps[(kt % 2) * 2 + 1]
                            P.mm(pc[:], [(sub(fc, fc.h[:, b, :]), sub(zt, zt.h[:, b, :])) for b in range(nb)])
                            P.mm(pS[:], [(sub(fs, fs.h[:, b, :]), sub(zt, zt.h[:, b, :])) for b in range(nb)])
                            gr = gre[kt % 2]
                            gi_ = gim[kt % 2]
                            P.tt(t1[:], pc[:], gr[:], ALU.mult)
                            P.tt(t2[:], pS[:], gi_[:], ALU.mult)
                            P.tt(sub(yre, yre.h[:, kt, :]), t1[:], t2[:], ALU.add, e="pool")
                            P.tt(t3[:], pS[:], gr[:], ALU.mult)
                            P.tt(t4[:], pc[:], gi_[:], ALU.mult)
                            P.tt(sub(yim, yim.h[:, kt, :]), t3[:], t4[:], ALU.subtract, e="pool")
                    with P.phase():
                        TW = 256
                        fct = [P.sb("fct%d" % i, [128, nb, TW], BF16) for i in range(3)]
                        fst = [P.sb("fst%d" % i, [128, nb, TW], BF16) for i in range(3)]
                        zs = [P.sb("zs%d" % i, [128, TW], F32) for i in range(2)]
                        x0s = [P.sb("x0s%d" % i, [128, TW], F32) for i in range(2)]
                        yo = [P.sb("yo%d" % i, [128, TW], BF16) for i in range(2)]
                        tmp = [P.sb("tmpc%d" % i, [128, TW], F32) for i in range(2)]
                        it = 0
                        for tt_ in range(Lc // TW):
                            fc = fct[tt_ % 3]
                            fs = fst[tt_ % 3]
                            P.dma("sp", fc[:], FcT_d.ref(FcT_d.h[tt_].rearrange("p (b t) -> p b t", b=nb)))
                            P.dma("sp", fs[:], FsT_d.ref(FsT_d.h[tt_].rearrange("p (b t) -> p b t", b=nb)))
                            c0 = tok0 + tt_ * TW
                            for cj in range(4):
                                pb = ps[it % 4]
                                o = sub(pb, pb.h[:, :TW])
                                P.mm(o, [(sub(yre, yre.h[:, kb, cj * 128:(cj + 1) * 128]), sub(fc, fc.h[:, kb, :])) for kb in range(nb)]
                                     + [(sub(yim, yim.h[:, kb, cj * 128:(cj + 1) * 128]), sub(fs, fs.h[:, kb, :])) for kb in range(nb)])
                                z_ = zs[it % 2]
                                x_ = x0s[it % 2]
                                P.dma("sp", z_[:], ZD.ref(ZD.h[cj * 128:(cj + 1) * 128, c0:c0 + TW]))
                                P.dma("sp", x_[:], X0D.ref(X0D.h[cj * 128:(cj + 1) * 128, c0:c0 + TW]))
                                t_ = tmp[it % 2]
                                P.ts(t_[:], z_[:], ppc(l, 96 + cj), ALU.mult, e="pool")
                                P.stt(t_[:], o, 1.0 / Lc, t_[:], ALU.mult, ALU.add)
                                P.tt(yo[it % 2][:], t_[:], x_[:], ALU.mult, e="pool")
                                P.dma("sp", YB[0].ref(YB[0].h[cj * 128:(cj + 1) * 128, c0:c0 + TW]), yo[it % 2][:])
                                it += 1

            with P.phase():
                bt = P.sb("bt", [64, 8, 15 * 64], BF16)
                with P.phase():
                    bst = P.sb("bst", [64, 8 * 15 * 64], F32)
                    P.dma("sp", bst[:], rpbx[l])
                    P.amul(sub(bt, bt.h[:].rearrange("p h x -> p (h x)")), bst[:], 8.0)
                vall = P.sb("vall", [64, 64, 512], BF16)
                for r0 in range(0, 64, 16):
                    P.dma("sp", sub(vall, vall.h[:, r0:r0 + 16, :]), VT.ref(VT.h[r0 * 64:(r0 + 16) * 64, :].rearrange("(r p) f -> p r f", p=64)))
                vctx = P.sb("vctx", [128, 2, 512], BF16)
                P.dma("sp", vctx[:], VT.ref(VT.h[NLAT:T, :].rearrange("(b p) f -> p b f", p=128)))
                qst = P.sb("qst", [64, T], F32)
                qTs = [P.sb("qT%d" % i, [64, T], BF16) for i in range(2)]
                kTs = [P.sb("kT%d" % i, [64, T], BF16) for i in range(2)]
                ring = [P.sb("pk%d" % i, [64, 768], BF16) for i in range(32)]
                pctxs = [P.sb("pctx%d" % i, [128, 2, 512], BF16) for i in range(2)]
                rec = P.sb("rec", [64, 512], F32)
                ynas = [P.sb("yna%d" % i, [64, 512], BF16) for i in range(2)]
                id64 = sub(identb, identb.h[0:64, 0:64])
                rlo = lambda kr: 0 if kr <= 7 else kr - 3
                rhi = lambda kr: 63 if kr >= 56 else kr + 4
                rsf = lambda r: min(max(r - 4, 0), 56)
                blk = 0
                sci = 0
                for h in range(8):
                    qT = qTs[h % 2]
                    kT = kTs[h % 2]
                    P.dma("sp", qst[:], PD.ref(PD.h[OFF_NA + h * 64:OFF_NA + (h + 1) * 64, :]))
                    P.copy(qT[:], qst[:], e="dve")
                    P.dma("sp", qst[:], PD.ref(PD.h[OFF_NA + 512 + h * 64:OFF_NA + 512 + (h + 1) * 64, :]))
                    P.copy(kT[:], qst[:], e="act")
                    done_kr = -1
                    for qb in range(9):
                        lat = qb < 8
                        W = 512 if lat else 256
                        q0 = qb * 512
                        if lat:
                            kmin = rsf(8 * qb)
                            kmax = rsf(8 * qb + 7) + 7
                            for kr in range(done_kr + 1, kmax + 1):
                                a, b_ = rlo(kr), rhi(kr)
                                pk = ring[kr % 32]
                                ca = a
                                while ca <= b_:
                                    cb = min(b_, ca + 7)
                                    wN = (cb - ca + 1) * 64
                                    pS = ps[sci % 2]
                                    sci += 1
                                    oap = pS.h[0:64, 0:wN]
                                    P.mmx([(oap, sub(kT, kT.h[:, kr * 64:(kr + 1) * 64]), sub(qT, qT.h[:, ca * 64:(cb + 1) * 64]), True, False),
                                           (oap, id64, sub(bt, bt.h[:, h, (7 - kr + ca) * 64:(7 - kr + cb + 1) * 64]), False, True)],
                                          [sub(pS, oap)])
                                    P.act(sub(pk, pk.h[:, (ca - a) * 64:(cb - a + 1) * 64]), sub(pS, oap), AF.Exp, scale=0.125)
                                    ca = cb + 1
                            done_kr = max(done_kr, kmax)
                        pctx = pctxs[blk % 2]
                        qblk = sub(qT, qT.h[:, q0:q0 + W])
                        for t in range(2):
                            pB = ps[2 + t]
                            P.mmx([(pB.h[:, 0:W], sub(kT, kT.h[:, NLAT + t * 128:NLAT + (t + 1) * 128]), qblk, True, True)], [sub(pB, pB.h[:, 0:W])])
                            P.act(sub(pctx, pctx.h[:, t, 0:W]), sub(pB, pB.h[:, 0:W]), AF.Exp, scale=0.125)
                        pO = ps[4 + blk % 2]
                        pD = ps[6 + blk % 2]
                        loc = []
                        if lat:
                            for kr in range(kmin, kmax + 1):
                                ra = max(rlo(kr), 8 * qb)
                                rb_ = min(rhi(kr), 8 * qb + 7)
                                if ra > rb_:
                                    continue
                                pk = ring[kr % 32]
                                loc.append(((ra - 8 * qb) * 64, (rb_ - 8 * qb + 1) * 64, kr,
                                            sub(pk, pk.h[:, (ra - rlo(kr)) * 64:(rb_ - rlo(kr) + 1) * 64])))
                        for (pX, isden) in ((pO, False), (pD, True)):
                            steps = []
                            c0 = sub(pctx, pctx.h[:, 0, 0:W])
                            c1 = sub(pctx, pctx.h[:, 1, 0:W])
                            l0 = onesb[:] if isden else sub(vctx, vctx.h[:, 0, h * 64:(h + 1) * 64])
                            l1 = onesb[:] if isden else sub(vctx, vctx.h[:, 1, h * 64:(h + 1) * 64])
                            steps.append((pX.h[0:64, 0:W], l0, c0, True, False))
                            for (x0, x1, kr, pr_) in loc:
                                lv = sub(onesb, onesb.h[0:64, :]) if isden else sub(vall, vall.h[:, kr, h * 64:(h + 1) * 64])
                                steps.append((pX.h[0:64, x0:x1], lv, pr_, False, False))
                            steps.append((pX.h[0:64, 0:W], l1, c1, False, True))
                            P.mmx(steps, [sub(pX, pX.h[0:64, 0:W])])
                        P.recip(sub(rec, rec.h[:, :W]), sub(pD, pD.h[0:64, :W]))
                        yn = ynas[blk % 2]
                        P.tt(sub(yn, yn.h[:, :W]), sub(pO, pO.h[0:64, :W]), sub(rec, rec.h[:, :W]), ALU.mult)
                        P.dma("sp", YB[2].ref(YB[2].h[h * 64:(h + 1) * 64, q0:q0 + W]), sub(yn, yn.h[:, :W]))
                        blk += 1

            with P.phase():
                wpr = P.sb("wpr", [128, 3, 4, D], BF16)
                wo = P.sb("wo", [128, 8, D], BF16)
                wst = [P.sb("wst%d" % i, [128, D], F32) for i in range(2)]
                i = 0
                for br in range(3):
                    for kc in range(4):
                        s_ = wst[i % 2]
                        P.dma("sp", s_[:], projs[br].ref(projs[br].h[l, kc * 128:(kc + 1) * 128, :]))
                        cast(sub(wpr, wpr.h[:, br, kc, :]), s_[:])
                        i += 1
                for kc in range(8):
                    s_ = wst[i % 2]
                    P.dma("sp", s_[:], w_o.ref(w_o.h[l, kc * 128:(kc + 1) * 128, :]))
                    cast(sub(wo, wo.h[:, kc, :]), s_[:])
                    i += 1
                ybs = [P.sb("yb%d" % i, [128, 3, 4, 512], BF16) for i in range(2)]
                xt = P.sb("xt", [128, 8, 512], F32)
                mg = P.sb("mg", [128, 8, 512], BF16)
                ya = P.sb("ya", [128, 8, 512], F32)
                sq = P.sb("sq", [128, 8, 512], F32)
                mean = P.sb("mean", [128, 512], F32)
                rstd = P.sb("rstd", [128, 512], F32)
                gts = [P.sb("gt%d" % i, [128, 512], F32) for i in range(4)]
                acc = P.sb("acc", [128, 512], F32)
                tm = [P.sb("tm%d" % i, [128, 512], F32) for i in range(2)]
                gi = 0
                for ti, (t0, n) in enumerate(TT):
                    kind = 0 if ti < 8 else 1
                    yb = ybs[ti % 2]
                    for br in range(3):
                        P.dma("sp", sub(yb, yb.h[:, br, :, :n]), YB[br].ref(YB[br].h[:, t0:t0 + n].rearrange("(kc p) t -> p kc t", p=128)))
                    for kc in range(8):
                        P.dma("sp", sub(xt, xt.h[:, kc, :n]), XT.ref(XT.h[kc * 128:(kc + 1) * 128, t0:t0 + n]))
                    for m in range(8):
                        for br in range(3):
                            pb = ps[2 + br]
                            o = sub(pb, pb.h[:, :n])
                            P.mm(o, [(sub(wpr, wpr.h[:, br, kc, m * 128:(m + 1) * 128]), sub(yb, yb.h[:, br, kc, :n])) for kc in range(4)])
                            g_ = gts[gi % 4]
                            gi += 1
                            gr = sub(g_, g_.h[:, :n])
                            r0 = OFF_GATE + br * D + m * 128
                            P.dma("sp", gr, PD.ref(PD.h[r0:r0 + 128, t0:t0 + n]))
                            P.act(gr, gr, AF.Sigmoid)
                            if br == 0:
                                P.tt(sub(acc, acc.h[:, :n]), o, gr, ALU.mult)
                            elif br == 1:
                                P.tt(sub(tm[0], tm[0].h[:, :n]), o, gr, ALU.mult)
                                P.tt(sub(acc, acc.h[:, :n]), sub(acc, acc.h[:, :n]), sub(tm[0], tm[0].h[:, :n]), ALU.add)
                            else:
                                P.tt(sub(tm[1], tm[1].h[:, :n]), o, gr, ALU.mult)
                                P.tt(sub(mg, mg.h[:, m, :n]), sub(acc, acc.h[:, :n]), sub(tm[1], tm[1].h[:, :n]), ALU.add)
                    for m in range(8):
                        pb = ps[5 + m % 2]
                        o = sub(pb, pb.h[:, :n])
                        P.mm(o, [(sub(wo, wo.h[:, kc, m * 128:(m + 1) * 128]), sub(mg, mg.h[:, kc, :n])) for kc in range(8)])
                        xm = sub(xt, xt.h[:, m, :n])
                        P.amul(xm, xm, ALPHA)
                        P.stt(sub(ya, ya.h[:, m, :n]), o, mod(l, 2, m, kind), xm, ALU.mult, ALU.add)
                    layer_norm(ya, n, l, 112, 120, (sq, mean, rstd))
                    for kc in range(8):
                        P.dma("sp", XT.ref(XT.h[kc * 128:(kc + 1) * 128, t0:t0 + n]), sub(ya, ya.h[:, kc, :n]))
                if "x1" in dbg:
                    pass

            for tiles in ((0, 1, 2), (3, 4, 5), (6, 7, 8)):
                with P.phase():
                    tl = [TT[i] for i in tiles]
                    base = tl[0][0]
                    ntok = sum(n for _, n in tl)
                    hb = P.sb("hb", [128, 8, ntok], BF16)
                    acc = P.sb("acc", [128, 8, ntok], F32)
                    wgt = P.sb("wgt", [64, ntok], F32)
                    with P.phase():
                        wr = P.sb("wr", [128, 8, 64], F32)
                        P.dma("sp", wr[:], moe_router.ref(moe_router.h[l].rearrange("(kc p) e -> p kc e", p=128)))
                        xt = P.sb("xt", [128, 8, 512], F32)
                        hf = P.sb("hf", [128, 8, 512], F32)
                        rt = {n_: P.sb("rt_" + n_, [128, 256], F32) for n_ in ("sc", "sl", "sl2", "slm", "em", "wv", "wg")}
                        r8 = {n_: P.sb("r8_" + n_, [128, 32], F32) for n_ in ("m1", "m2", "gs", "s8", "gm", "pen", "t8")}
                        r1 = {n_: P.sb("r1_" + n_, [128, 4], F32) for n_ in ("ss", "rs")}
                        for (t0, n) in tl:
                            off = t0 - base
                            kind = 0 if t0 < NLAT else 1
                            for kc in range(8):
                                P.dma("sp", sub(xt, xt.h[:, kc, :n]), XT.ref(XT.h[kc * 128:(kc + 1) * 128, t0:t0 + n]))
                            for kc in range(8):
                                P.act(sub(hf, hf.h[:, kc, :n]), sub(xt, xt.h[:, kc, :n]), AF.Identity, bias=mod(l, 3, kc, kind), scale=mod(l, 4, kc, kind))
                                P.copy(sub(hb, hb.h[:, kc, off:off + n]), sub(hf, hf.h[:, kc, :n]), e="dve")
                            ns = n // 128
                            G = ns * 8
                            W = ns * 64
                            pr = sub(ps[7], ps[7].h[:, 0:W])
                            for s in range(ns):
                                P.mm(sub(ps[7], ps[7].h[:, s * 64:(s + 1) * 64]), [(sub(hf, hf.h[:, kc, s * 128:(s + 1) * 128]), sub(wr, wr.h[:, kc, :])) for kc in range(8)])
                            R_ = lambda t_, ap: Ref(ap, [t_.buf])
                            sc, sl, sl2, slm, em, wv, wg = (rt[k] for k in ("sc", "sl", "sl2", "slm", "em", "wv", "wg"))
                            m1, m2, gs, s8, gm, pen, t8 = (r8[k] for k in ("m1", "m2", "gs", "s8", "gm", "pen", "t8"))
                            g3 = lambda t_: R_(t_, t_.h[:, 0:W].rearrange("p (g e) -> p g e", e=8))
                            s3 = lambda t_: R_(t_, t_.h[:, 0:W].rearrange("p (s e) -> p s e", e=64))
                            P.act(R_(sc, sc.h[:, 0:W]), pr, AF.Sigmoid)
                            P.tt(s3(sl), s3(sc), Ref(biasb.h[:, l, :].unsqueeze(1).broadcast_to([128, ns, 64]), [biasb.buf]), ALU.add)
                            P.rmax(R_(m1, m1.h[:, 0:G]), g3(sl))
                            P.tt(g3(sl2), g3(sl), R_(m1, m1.h[:, 0:G].unsqueeze(2).broadcast_to([128, G, 8])), ALU.is_equal)
                            P.stt(R_(sl2, sl2.h[:, 0:W]), R_(sl2, sl2.h[:, 0:W]), -1e9, R_(sl, sl.h[:, 0:W]), ALU.mult, ALU.add)
                            P.rmax(R_(m2, m2.h[:, 0:G]), g3(sl2))
                            P.tt(R_(gs, gs.h[:, 0:G]), R_(m1, m1.h[:, 0:G]), R_(m2, m2.h[:, 0:G]), ALU.add)
                            for s in range(ns):
                                P.top8(R_(s8, s8.h[:, s * 8:(s + 1) * 8]), R_(gs, gs.h[:, s * 8:(s + 1) * 8]))
                            g2 = lambda t_: t_.h[:, 0:G].rearrange("p (s g) -> p s g", g=8)
                            P.tt(R_(gm, g2(gm)), R_(gs, g2(gs)), R_(s8, g2(s8)[:, :, 3:4].broadcast_to([128, ns, 8])), ALU.is_ge)
                            P.ts(R_(pen, pen.h[:, 0:G]), R_(gm, gm.h[:, 0:G]), 1e9, ALU.mult, -1e9, ALU.add)
                            P.tt(g3(slm), g3(sl), R_(pen, pen.h[:, 0:G].unsqueeze(2).broadcast_to([128, G, 8])), ALU.add)
                            for s in range(ns):
                                P.top8(R_(t8, t8.h[:, s * 8:(s + 1) * 8]), R_(slm, slm.h[:, s * 64:(s + 1) * 64]))
                            P.tt(s3(em), s3(slm), R_(t8, g2(t8)[:, :, 7:8].broadcast_to([128, ns, 64])), ALU.is_ge)
                            P.tt(R_(wv, wv.h[:, 0:W]), R_(sc, sc.h[:, 0:W]), R_(em, em.h[:, 0:W]), ALU.mult)
                            ss, rs_ = r1["ss"], r1["rs"]
                            P.rsum(R_(ss, ss.h[:, 0:ns]), s3(wv))
                            P.recip(R_(rs_, rs_.h[:, 0:ns]), R_(ss, ss.h[:, 0:ns]))
                            P.stt(s3(wg), s3(wv), 2.5, R_(rs_, rs_.h[:, 0:ns].unsqueeze(2).broadcast_to([128, ns, 64])), ALU.mult, ALU.mult)
                            for s in range(ns):
                                P.mm(sub(ps[6], ps[6].h[0:64, s * 128:(s + 1) * 128]), [(R_(wg, wg.h[:, s * 64:(s + 1) * 64]), identf[:])])
                            P.copy(sub(wgt, wgt.h[:, off:off + n]), sub(ps[6], ps[6].h[0:64, 0:n]), e="act")
                        P.dma("sp", WG.ref(WG.h[:, base:base + ntok]), wgt[:])
                    with P.phase():
                        w13s = P.sb("w13s", [128, 8, 512], F32)
                        w2st = P.sb("w2st", [128, 2, D], F32)
                        w13 = [[P.sb("w13_%d%d" % (i, j), [128, 8, 512], BF16) for j in range(2)] for i in range(2)]
                        w2b = [[P.sb("w2b_%d%d" % (i, j), [128, 2, D], BF16) for j in range(2)] for i in range(2)]
                        s1 = [P.sb("s1_%d" % i, [128, 512], F32) for i in range(2)]
                        t_ = [P.sb("t_%d" % i, [128, 512], F32) for i in range(2)]
                        hh = [[P.sb("hh%d%d" % (i, j), [128, 2, 512], BF16) for j in range(2)] for i in range(2)]
                        wsel = [P.sb("wsel%d" % i, [64, 512], F32) for i in range(2)]
                        wshi = [P.sb("wshi%d" % i, [64, 512], BF16) for i in range(2)]
                        wslo = [P.sb("wslo%d" % i, [64, 512], BF16) for i in range(2)]
                        po_i = [0]

                        gsb = [[P.sb("gsb%d%d" % (i, j), [128, 512], F32) for j in range(2)] for i in range(2)]

                        def GprepA(e, n, off, par, j):
                            if e < 0:
                                return
                            g_ = gsb[par][j]
                            P.dma("sp", sub(g_, g_.h[:, :n]), Ref(WG.h[e:e + 1, base + off:base + off + n].broadcast_to([128, n]), [], True))

                        def GprepB(e, n, off, par, j):
                            return

                        def S1(e, a13, n, off, hcur, j, par):
                            pG = sub(gsb[par][j], gsb[par][j].h[:, :n])
                            for fc in range(2):
                                p1 = sub(ps[fc * 2], ps[fc * 2].h[:, :n])
                                p3 = sub(ps[fc * 2 + 1], ps[fc * 2 + 1].h[:, :n])
                                P.mm(p1, [(sub(a13, a13.h[:, kc, fc * 128:(fc + 1) * 128]), sub(hb, hb.h[:, kc, off:off + n])) for kc in range(8)])
                                P.mm(p3, [(sub(a13, a13.h[:, kc, 256 + fc * 128:256 + (fc + 1) * 128]), sub(hb, hb.h[:, kc, off:off + n])) for kc in range(8)])
                                s_ = sub(s1[fc], s1[fc].h[:, :n])
                                P.act(s_, p1, AF.Silu)
                                if e >= 0:
                                    tr = sub(t_[fc], t_[fc].h[:, :n])
                                    P.tt(tr, p3, s_, ALU.mult)
                                    P.tt(sub(hcur, hcur.h[:, fc, :n]), tr, pG, ALU.mult)
                                else:
                                    P.tt(sub(hcur, hcur.h[:, fc, :n]), p3, s_, ALU.mult)

                        def S2(grp, a2s, n, off, hcs):
                            for m in range(8):
                                pO = ps[4 + po_i[0] % 4]
                                po_i[0] += 1
                                o = sub(pO, pO.h[:, :n])
                                P.mm(o, [(sub(a2s[j], a2s[j].h[:, fc, m * 128:(m + 1) * 128]), sub(hcs[j], hcs[j].h[:, fc, :n]))
                                         for j in range(len(grp)) for fc in range(2)])
                                ar = sub(acc, acc.h[:, m, off:off + n])
                                if grp[0] < 0:
                                    P.copy(ar, o, e="act")
                                else:
                                    P.tt(ar, ar, o, ALU.add)

                        groups = [[-1]] + [[2 * i, 2 * i + 1] for i in range(32)]
                        prev = None
                        it = 0
                        def wload(gi, j):
                            e = groups[gi][j]
                            a13 = w13[gi % 2][j]
                            a2 = w2b[gi % 2][j]
                            if e < 0:
                                srcs = (sh_w1.h[l], sh_w3.h[l], sh_w2.h[l])
                            else:
                                srcs = (moe_w1.h[l, e], moe_w3.h[l, e], moe_w2.h[l, e])
                            P.dma("sp", sub(w13s, w13s.h[:, :, 0:256]), Ref(srcs[0].rearrange("(kc p) f -> p kc f", p=128), [], True))
                            P.dma("sp", sub(w13s, w13s.h[:, :, 256:512]), Ref(srcs[1].rearrange("(kc p) f -> p kc f", p=128), [], True))
                            P.dma("sp", w2st[:], Ref(srcs[2].rearrange("(fc p) m -> p fc m", p=128), [], True))
                            P.copy(a13[:], w13s[:], e="act")
                            P.copy(a2[:], w2st[:], e="act")

                        wload(0, 0)
                        items = [(gi, grp, ti_, t0, n) for gi, grp in enumerate(groups) for ti_, (t0, n) in enumerate(tl)]
                        for k, (gi, grp, ti_, t0, n) in enumerate(items):
                            off = t0 - base
                            hcs = [hh[k % 2][j] for j in range(len(grp))]
                            nxt = items[k + 1] if k + 1 < len(items) else None
                            if nxt is not None:
                                for j, e in enumerate(nxt[1]):
                                    GprepA(e, nxt[4], nxt[3] - base, (k + 1) % 2, j)
                            for j, e in enumerate(grp):
                                S1(e, w13[gi % 2][j], n, off, hcs[j], j, k % 2)
                            if prev is not None:
                                S2(*prev)
                            if nxt is not None:
                                for j, e in enumerate(nxt[1]):
                                    GprepB(e, nxt[4], nxt[3] - base, (k + 1) % 2, j)
                            prev = (grp, [w2b[gi % 2][j] for j in range(len(grp))], n, off, hcs)
                            if gi + 1 < len(groups) and ti_ < len(groups[gi + 1]):
                                wload(gi + 1, ti_)
                        S2(*prev)
                    with P.phase():
                        xt = P.sb("xt", [128, 8, 512], F32)
                        ya = P.sb("ya", [128, 8, 512], F32)
                        mean = P.sb("mean", [128, 512], F32)
                        rstd = P.sb("rstd", [128, 512], F32)
                        for (t0, n) in tl:
                            off = t0 - base
                            kind = 0 if t0 < NLAT else 1
                            for kc in range(8):
                                P.dma("sp", sub(xt, xt.h[:, kc, :n]), XT.ref(XT.h[kc * 128:(kc + 1) * 128, t0:t0 + n]))
                            xv = sub(xt, xt.h[:, :, :n])
                            yv = sub(ya, ya.h[:, :, :n])
                            P.amul(xv, xv, ALPHA)
                            P.tt(yv, sub(acc, acc.h[:, :, off:off + n]),
                                 Ref(mods.h[:, l, 40:48, kind].unsqueeze(2).broadcast_to([128, 8, n]), [mods.buf]), ALU.mult)
                            P.tt(yv, yv, xv, ALU.add)
                            layer_norm(ya, n, l, 128, 136, (xt, mean, rstd))
                            dst = yT if (last and kind == 0) else XT
                            for kc in range(8):
                                P.dma("sp", dst.ref(dst.h[kc * 128:(kc + 1) * 128, t0:t0 + n]), sub(ya, ya.h[:, kc, :n]))

        if "xt" in dbg:
            xdbg = P.dram("xdbg", [D, T], F32, kind="ExternalOutput")
            with P.phase():
                xs = [P.sb("xfin%d" % i, [128, T], F32) for i in range(2)]
                for kc in range(8):
                    x_ = xs[kc % 2]
                    P.dma("sp", x_[:], XT.ref(XT.h[kc * 128:(kc + 1) * 128, :]))
                    P.dma("sp", xdbg.ref(xdbg.h[kc * 128:(kc + 1) * 128, :]), x_[:])
        P.barrier()
        print("build: ninst", P.ninst, "nwait", P.nwait, "nsem", len(P.allsems) + 4)
    return nc


_CONST = {}


def _consts():
    if _CONST:
        return _CONST
    bf = ml_dtypes.bfloat16

    def dft(Lc, TW):
        N = 2 * Lc
        t = np.arange(Lc, dtype=np.int64)
        k = np.arange(Lc, dtype=np.int64)
        j = (t[:, None] * (2 * k[None, :] + 1)) % (2 * N)
        th = j.astype(np.float64) * (2.0 * np.pi / (2 * N))
        c = np.cos(th)
        s = np.sin(th)
        nb = Lc // 128

        def fwd(m):
            return np.ascontiguousarray(m.reshape(nb, 128, nb, 128).transpose(2, 1, 0, 3).reshape(nb, 128, nb * 128)).astype(bf)

        def inv(m):
            mt = m.T
            ntt = Lc // TW
            return np.ascontiguousarray(mt.reshape(nb, 128, ntt, TW).transpose(2, 1, 0, 3).reshape(ntt, 128, nb * TW)).astype(bf)
        return fwd(c), fwd(s), inv(c), inv(s)

    def feats(Lc):
        f32 = np.float32
        t = np.linspace(0.0, 1.0, Lc, dtype=f32)[:, None]
        ang = (f32(2.0 * math.pi / Lc)) * np.arange(Lc, dtype=f32)[:, None]
        bands = np.linspace(1e-4, 15, 16, dtype=f32)[None, :]
        fe = np.concatenate([t, np.cos(bands * ang), -np.sin(bands * ang)], -1).astype(f32)
        deltas = np.abs(np.linspace(math.log(1e-2) / 1.5, math.log(1e-2) / 0.3, 512, dtype=f32))
        dec = np.exp(-t * deltas[None, :]).astype(f32)
        return np.ascontiguousarray(fe.T), dec

    _CONST["Fc"], _CONST["Fs"], _CONST["FcT"], _CONST["FsT"] = dft(NLAT, 256)
    _CONST["cFc"], _CONST["cFs"], _CONST["cFcT"], _CONST["cFsT"] = dft(NCTX, 256)
    _CONST["featsT"], _CONST["decay"] = feats(NLAT)
    _CONST["cfeatsT"], _CONST["cdecay"] = feats(NCTX)
    _CONST["ident"] = np.eye(128, dtype=np.float32)
    return _CONST


def _rpb_expand(na_rpb):
    Lr = na_rpb.shape[0]
    c = np.arange(64)
    cs = np.clip(c - 8, 0, 48)
    kc = np.arange(64)
    inwin = (kc[:, None] >= cs[None, :]) & (kc[:, None] < cs[None, :] + 16)
    rel = np.clip(kc[:, None] - c[None, :] + 15, 0, 30)
    g = na_rpb[:, :, :, rel]
    g = np.where(inwin[None, None, None], g, np.float32(-1e30)).astype(np.float32)
    g = g[:, :, ::-1]
    return np.ascontiguousarray(g.transpose(0, 3, 1, 2, 4)).reshape(Lr, 64, 8 * 15 * 64)


def _pack_pp(inp, nl):
    pp = np.zeros((nl, 128, NPP), np.float32)

    def chunks(v):
        return v.reshape(-1, 128).T
    for l in range(nl):
        pp[l, :, 0:48] = chunks(inp["b_ada"][l])
        cw = inp["hy_conv_w"][l]
        for c12 in range(12):
            for tap in range(3):
                pp[l, :, 48 + c12 * 3 + tap] = cw[tap, c12 * 128:(c12 + 1) * 128]
        pp[l, :, 84:96] = chunks(inp["hy_conv_b"][l])
        pp[l, :, 96:100] = chunks(inp["hy_bias_d"][l])
        sw = inp["sc_conv_w"][l]
        for j in range(4):
            for tap in range(3):
                pp[l, :, 100 + j * 3 + tap] = sw[tap, j * 128:(j + 1) * 128]
        pp[l, :, 112:120] = chunks(inp["ln1_g"][l])
        pp[l, :, 120:128] = chunks(inp["ln1_b"][l])
        pp[l, :, 128:136] = chunks(inp["ln2_g"][l])
        pp[l, :, 136:144] = chunks(inp["ln2_b"][l])
        pp[l, 0:64, 144] = inp["hy_b1"][l]
        pp[l, 0:64, 145] = inp["hy_b2"][l]
        pp[l, 0:64, 146] = inp["hy_sin_freq"][l, 0]
        pp[l, 0:64, 147] = inp["hy_sin_freq"][l, 1]
    return pp


def make_in_maps(inp, cores, nl=DEPTH):
    cst = _consts()
    f = lambda a: np.ascontiguousarray(np.asarray(a, dtype=np.float32))
    shared = {
        "w_ada": f(inp["w_ada"][:nl]), "pp": _pack_pp(inp, nl), "w_in": f(inp["w_in"][:nl]),
        "hy_w1": f(inp["hy_w1"][:nl]), "hy_w2": f(inp["hy_w2"][:nl]), "hy_w3": f(inp["hy_w3"][:nl]),
        "hy_proj": f(inp["hy_proj"][:nl]), "sc_proj": f(inp["sc_proj"][:nl]), "na_proj": f(inp["na_proj"][:nl]),
        "w_o": f(inp["w_o"][:nl]), "rpbx": _rpb_expand(np.asarray(inp["na_rpb"][:nl], np.float32)),
        "moe_router": f(inp["moe_router"][:nl]),
        "moe_biasb": np.ascontiguousarray(np.broadcast_to(np.asarray(inp["moe_bias"][:nl], np.float32)[:, None, :], (nl, 128, 64))),
        "moe_w1": f(inp["moe_w1"][:nl]), "moe_w3": f(inp["moe_w3"][:nl]), "moe_w2": f(inp["moe_w2"][:nl]),
        "sh_w1": f(inp["sh_w1"][:nl]), "sh_w3": f(inp["sh_w3"][:nl]), "sh_w2": f(inp["sh_w2"][:nl]),
    }
    shared.update(cst)
    cc = np.asarray(inp["c_ctx"], np.float32).reshape(8, 128).T
    maps = []
    for b in cores:
        m = dict(shared)
        m["xT"] = np.ascontiguousarray(np.concatenate([np.asarray(inp["x"][b], np.float32).T, np.asarray(inp["ctx"][b], np.float32).T], axis=1))
        cb = np.asarray(inp["c"][b], np.float32).reshape(8, 128).T
        m["cvec"] = np.ascontiguousarray(np.stack([cb, cc], axis=-1))
        maps.append(m)
    return maps


_NC = {}


def kernel(**inputs):
    inp = {k: np.asarray(v) for k, v in inputs.items()}
    if DEPTH not in _NC:
        _NC[DEPTH] = build(DEPTH)
    nc = _NC[DEPTH]
    maps = make_in_maps(inp, list(range(8)))
    res = run_bass_kernel_spmd(nc, maps, core_ids=list(range(8)))
    out = np.stack([np.ascontiguousarray(res.results[b]["yT"].T) for b in range(8)], axis=0)
    return out.astype(np.float32)
```

```python
import math
from contextlib import ExitStack
import numpy as np
import ml_dtypes
import concourse.bass as bass
import concourse.mybir as mybir
from concourse.bass_utils import run_bass_kernel_spmd

F32 = mybir.dt.float32
BF16 = mybir.dt.bfloat16
AF = mybir.ActivationFunctionType
ALU = mybir.AluOpType
AX = mybir.AxisListType

D = 1024
NLAT = 4096
NCTX = 256
T = NLAT + NCTX
DEPTH = 4
OFF_HY, OFF_SC, OFF_NA, OFF_GATE, DIN = 0, 1536, 3072, 4608, 7680
TT = [(i * 512, 512) for i in range(8)] + [(4096, 256)]
ALPHA = (2.0 * DEPTH) ** 0.25
LN_EPS = 1e-5
NPP = 160
MAGIC = 12582912.0
TWO_PI = 2.0 * math.pi


class Buf:
    __slots__ = ("name", "w", "r", "sem", "cnt")

    def __init__(self, name):
        self.name = name
        self.w = {}
        self.r = {}
        self.sem = None
        self.cnt = 0


class Ref:
    __slots__ = ("ap", "bufs", "dram")

    def __init__(self, ap, bufs, dram=False):
        self.ap = ap
        self.bufs = bufs
        self.dram = dram


class Tile:
    def __init__(self, handle, name, dram=False):
        self.h = handle
        self.buf = Buf(name)
        self.dram = dram

    def __getitem__(self, idx):
        return Ref(self.h[idx], [] if self.dram else [self.buf], self.dram)

    def ref(self, ap):
        return Ref(ap, [] if self.dram else [self.buf], self.dram)


class Prog:
    def __init__(self, nc, stack):
        self.nc = nc
        self.gstack = stack
        self.stack = stack
        self.eng = {"pe": nc.tensor, "act": nc.scalar, "dve": nc.vector, "pool": nc.gpsimd, "sp": nc.sync}
        self.sem = {}
        self.cnt = {}
        for e in ("pe", "act", "dve", "pool"):
            self.sem[e] = stack.enter_context(nc.semaphore("s_" + e))
            self.cnt[e] = 0
        self.waited = {e: {} for e in self.eng}
        self.sempool = []
        self.allsems = []
        self.phase_bufs = []
        self.ninst = 0
        self.nwait = 0
        self.uid = 0

    def sb(self, name, shape, dt):
        self.uid += 1
        h = self.stack.enter_context(self.nc.sbuf_tensor("%s_%d" % (name, self.uid), list(shape), dt))
        t = Tile(h, name)
        self.phase_bufs.append(t.buf)
        return t

    def ps(self, name, shape, dt=F32):
        h = self.gstack.enter_context(self.nc.psum_tensor(name, list(shape), dt))
        return Tile(h, name)

    def dram(self, name, shape, dt, kind="Internal"):
        h = self.nc.dram_tensor(name, list(shape), dt, kind=kind)
        return Tile(h, name, dram=True)

    def _dsem(self, buf):
        if buf.sem is None:
            if self.sempool:
                ent = self.sempool.pop()
            else:
                ent = [self.gstack.enter_context(self.nc.semaphore("d%d" % len(self.allsems))), 0]
                self.allsems.append(ent)
            buf.sem = ent
            buf.cnt = ent[1]
        return buf.sem[0]

    class _Phase:
        def __init__(self, P):
            self.P = P

        def __enter__(self):
            P = self.P
            self.prev_stack = P.stack
            self.prev_bufs = P.phase_bufs
            self.es = ExitStack()
            self.es.__enter__()
            P.stack = self.es
            P.phase_bufs = []
            return self

        def __exit__(self, *a):
            P = self.P
            P.barrier()
            for b in P.phase_bufs:
                if b.sem is not None:
                    b.sem[1] = b.cnt
                    P.sempool.append(b.sem)
                    b.sem = None
                if b in P._live_set:
                    P._live_set.discard(b)
            P._live = [b for b in P._live if b in P._live_set]
            P.stack = self.prev_stack
            P.phase_bufs = self.prev_bufs
            self.es.__exit__(None, None, None)
            return False

    def phase(self):
        return Prog._Phase(self)

    def barrier(self):
        toks = [(self.sem[e], self.cnt[e]) for e in self.sem if self.cnt[e] > 0]
        live = {}
        for b in self._all_live_bufs():
            if b.sem is not None and b.cnt > 0:
                live[id(b.sem[0])] = (b.sem[0], b.cnt)
        toks += list(live.values())
        for e, en in self.eng.items():
            wd = self.waited[e]
            for s, v in toks:
                if wd.get(id(s), 0) < v:
                    en.wait_ge(s, v)
                    wd[id(s)] = v
                    self.nwait += 1

    def _all_live_bufs(self):
        return self._live

    def _deps(self, e, reads, writes):
        deps = {}
        for b in reads:
            for k, (s, v) in b.w.items():
                if deps.get(k, (None, 0))[1] < v:
                    deps[k] = (s, v)
        for b in writes:
            for d in (b.w, b.r):
                for k, (s, v) in d.items():
                    if deps.get(k, (None, 0))[1] < v:
                        deps[k] = (s, v)
        wd = self.waited[e]
        en = self.eng[e]
        own = id(self.sem["pe"]) if e == "pe" else None
        for k, (s, v) in deps.items():
            if k == own:
                continue
            if wd.get(k, 0) < v:
                en.wait_ge(s, v)
                wd[k] = v
                self.nwait += 1

    def _done(self, tok, reads, writes):
        k = id(tok[0])
        for b in writes:
            b.w = {k: tok}
            b.r = {}
        for b in reads:
            if b.r.get(k, (None, 0))[1] < tok[1]:
                b.r[k] = tok

    def op(self, e, fn, reads, writes):
        rb = [b for r in reads for b in r.bufs]
        wb = [b for r in writes for b in r.bufs]
        self._deps(e, rb, wb)
        ins = fn(self.eng[e])
        self.cnt[e] += 1
        ins.then_inc(self.sem[e], 1)
        self.ninst += 1
        self._done((self.sem[e], self.cnt[e]), rb, wb)
        return ins

    def mmx(self, steps, writes):
        rb = [b for st in steps for x in st[1:3] for b in x.bufs]
        wb = [b for r in writes for b in r.bufs]
        self._deps("pe", rb, wb)
        ins = None
        en = self.eng["pe"]
        for (o, l, r, s0, s1) in steps:
            ins = en.matmul(o, l.ap, r.ap, start=s0, stop=s1)
            self.ninst += 1
        self.cnt["pe"] += 1
        ins.then_inc(self.sem["pe"], 1)
        self._done((self.sem["pe"], self.cnt["pe"]), rb, wb)

    def mm(self, out, pairs):
        n = len(pairs)
        self.mmx([(out.ap, l, r, i == 0, i == n - 1) for i, (l, r) in enumerate(pairs)], [out])

    def dma(self, q, out, in_, **kw):
        if out.dram and q == "sp":
            q = "pool"
        rb = list(in_.bufs)
        wb = list(out.bufs)
        self._deps(q, rb, wb)
        side = out.bufs[0] if not out.dram else in_.bufs[0]
        s = self._dsem(side)
        if side not in self._live_set:
            self._live_set.add(side)
            self._live.append(side)
        ins = self.eng[q].dma_start(out=out.ap, in_=in_.ap, **kw)
        side.cnt += 16
        ins.then_inc(s, 16)
        self.ninst += 1
        self._done((s, side.cnt), rb, wb)

    def act(self, out, in_, func, bias=None, scale=1.0):
        reads = [in_]
        kw = {}
        if isinstance(bias, Ref):
            reads.append(bias)
            kw["bias"] = bias.ap
        elif bias is not None:
            kw["bias"] = bias
        if isinstance(scale, Ref):
            reads.append(scale)
            kw["scale"] = scale.ap
        else:
            kw["scale"] = scale
        return self.op("act", lambda en: en.activation(out.ap, in_.ap, func, **kw), reads, [out])

    def amul(self, out, in_, m):
        reads = [in_]
        v = m
        if isinstance(m, Ref):
            reads.append(m)
            v = m.ap
        return self.op("act", lambda en: en.mul(out.ap, in_.ap, v), reads, [out])

    def tt(self, out, a, b, op, e="dve"):
        return self.op(e, lambda en: en.tensor_tensor(out.ap, a.ap, b.ap, op), [a, b], [out])

    def ts(self, out, a, s1, op0, s2=None, op1=None, e="dve"):
        reads = [a]
        v1 = s1
        if isinstance(s1, Ref):
            reads.append(s1)
            v1 = s1.ap
        v2 = s2
        if isinstance(s2, Ref):
            reads.append(s2)
            v2 = s2.ap
        kw = {}
        if op1 is not None:
            kw["op1"] = op1
        return self.op(e, lambda en: en.tensor_scalar(out.ap, a.ap, v1, v2, op0, **kw), reads, [out])

    def stt(self, out, a, s, b, op0, op1, e="dve"):
        e = "dve"
        reads = [a, b]
        v = s
        if isinstance(s, Ref):
            reads.append(s)
            v = s.ap
        return self.op(e, lambda en: en.scalar_tensor_tensor(out.ap, a.ap, v, b.ap, op0, op1), reads, [out])

    def copy(self, out, in_, e="dve"):
        if e == "act":
            return self.op("act", lambda en: en.copy(out.ap, in_.ap), [in_], [out])
        return self.op(e, lambda en: en.tensor_copy(out.ap, in_.ap), [in_], [out])

    def memset(self, out, val, e="pool"):
        return self.op(e, lambda en: en.memset(out.ap, val), [], [out])

    def recip(self, out, in_):
        return self.op("dve", lambda en: en.reciprocal(out.ap, in_.ap), [in_], [out])

    def rmax(self, out, in_):
        return self.op("dve", lambda en: en.tensor_reduce(out.ap, in_.ap, AX.X, ALU.max), [in_], [out])

    def rsum(self, out, in_):
        return self.op("dve", lambda en: en.tensor_reduce(out.ap, in_.ap, AX.X, ALU.add), [in_], [out])

    def top8(self, out, in_):
        return self.op("dve", lambda en: en.max(out.ap, in_.ap), [in_], [out])


def build(nlay=DEPTH, dbg=()):
    nc = bass.Bass("TRN2", target_bir_lowering=False)
    st = ExitStack()
    with st:
        P = Prog(nc, st)
        P._live = []
        P._live_set = set()
        L = nlay
        ext = lambda n, s, dt=F32: P.dram(n, s, dt, kind="ExternalInput")
        xT_in = ext("xT", [D, T])
        cvec = ext("cvec", [128, 8, 2])
        w_ada = ext("w_ada", [L, D, 6 * D])
        pp_d = ext("pp", [L, 128, NPP])
        w_in = ext("w_in", [L, D, DIN])
        hy_w1 = ext("hy_w1", [L, 33, 64])
        hy_w2 = ext("hy_w2", [L, 64, 64])
        hy_w3 = ext("hy_w3", [L, 64, 1024])
        projs = [ext(n, [L, 512, D]) for n in ("hy_proj", "sc_proj", "na_proj")]
        w_o = ext("w_o", [L, D, D])
        rpbx = ext("rpbx", [L, 64, 8 * 15 * 64])
        moe_router = ext("moe_router", [L, D, 64])
        moe_biasb = ext("moe_biasb", [L, 128, 64])
        moe_w1 = ext("moe_w1", [L, 64, D, 256])
        moe_w3 = ext("moe_w3", [L, 64, D, 256])
        moe_w2 = ext("moe_w2", [L, 64, 256, D])
        sh_w1 = ext("sh_w1", [L, D, 256])
        sh_w3 = ext("sh_w3", [L, D, 256])
        sh_w2 = ext("sh_w2", [L, 256, D])
        Fc = ext("Fc", [32, 128, 32 * 128], BF16)
        Fs = ext("Fs", [32, 128, 32 * 128], BF16)
        FcT = ext("FcT", [16, 128, 32 * 256], BF16)
        FsT = ext("FsT", [16, 128, 32 * 256], BF16)
        cFc = ext("cFc", [2, 128, 2 * 128], BF16)
        cFs = ext("cFs", [2, 128, 2 * 128], BF16)
        cFcT = ext("cFcT", [1, 128, 2 * 256], BF16)
        cFsT = ext("cFsT", [1, 128, 2 * 256], BF16)
        featsT = ext("featsT", [33, NLAT])
        cfeatsT = ext("cfeatsT", [33, NCTX])
        decay = ext("decay", [NLAT, 512])
        cdecay = ext("cdecay", [NCTX, 512])
        ident_d = ext("ident", [128, 128])
        yT = P.dram("yT", [D, NLAT], F32, kind="ExternalOutput")
        dbg_out = {}

        XT = P.dram("XT", [D, T], F32)
        PD = P.dram("PD", [DIN, T], F32)
        VT = P.dram("VT", [T, 512], BF16)
        ZD = P.dram("ZD", [512, T], F32)
        X0D = P.dram("X0D", [512, T], F32)
        ZT = P.dram("ZT", [T, 512], BF16)
        YB = [P.dram(n, [512, T], BF16) for n in ("YHY", "YSC", "YNA")]
        HTD = P.dram("HTD", [NLAT, 1024], BF16)
        cHTD = P.dram("cHTD", [NCTX, 1024], BF16)
        GRE = P.dram("GRE", [NLAT, 512], F32)
        GIM = P.dram("GIM", [NLAT, 512], F32)
        cGRE = P.dram("cGRE", [NCTX, 512], F32)
        cGIM = P.dram("cGIM", [NCTX, 512], F32)
        WG = P.dram("WG", [64, T], F32)

        ps = [P.ps("ps%d" % i, [128, 512]) for i in range(8)]

        ppall = P.sb("ppall", [128, L, NPP], F32)
        mods = P.sb("mods", [128, L, 48, 2], F32)
        identf = P.sb("identf", [128, 128], F32)
        identb = P.sb("identb", [128, 128], BF16)
        onesf = P.sb("onesf", [128, 128], F32)
        onesb = P.sb("onesb", [128, 64], BF16)
        onesb2 = P.sb("onesb2", [128, 128], BF16)
        biasb = P.sb("biasb", [128, L, 64], F32)

        def ppc(l, c, n=1, p0=0, p1=128):
            return Ref(ppall.h[p0:p1, l, c:c + n], [ppall.buf])

        def mod(l, which, kc, kind):
            return Ref(mods.h[:, l, which * 8 + kc, kind:kind + 1], [mods.buf])

        def sub(t, ap):
            return Ref(ap, [t.buf])

        with P.phase():
            for l in range(L):
                P.dma("sp", Ref(ppall.h[:, l, :], [ppall.buf]), pp_d[l])
                P.dma("sp", Ref(biasb.h[:, l, :], [biasb.buf]), moe_biasb[l])
            P.dma("sp", identf[:], ident_d[:])
            P.copy(identb[:], identf[:])
            P.memset(onesf[:], 1.0)
            P.memset(onesb[:], 1.0)
            P.memset(onesb2[:], 1.0)
            cv = P.sb("cv", [128, 8, 2], F32)
            P.dma("sp", cv[:], cvec[:])
            cond = P.sb("cond", [128, 8, 2], F32)
            P.act(cond[:], cv[:], AF.Silu)
            was = [P.sb("wa%d" % i, [128, 8, 1024], F32) for i in range(2)]
            i = 0
            for l in range(L):
                for mb in range(6):
                    wa = was[i % 2]
                    for kc in range(8):
                        P.dma("sp", sub(wa, wa.h[:, kc, :]), w_ada.ref(w_ada.h[l, kc * 128:(kc + 1) * 128, mb * 1024:(mb + 1) * 1024]))
                    for mi in range(8):
                        m = mb * 8 + mi
                        pb = ps[mi % 4]
                        o = sub(pb, pb.h[:, 0:2])
                        P.mm(o, [(sub(wa, wa.h[:, kc, mi * 128:(mi + 1) * 128]), sub(cond, cond.h[:, kc, :])) for kc in range(8)])
                        P.ts(sub(mods, mods.h[:, l, m, :]), o, ppc(l, m), ALU.add)
                    i += 1
                for w0 in (8, 32):
                    r = sub(mods, mods.h[:, l, w0:w0 + 8, :])
                    P.ts(r, r, 1.0, ALU.add)
            xs = [P.sb("xinit%d" % i, [128, T], F32) for i in range(2)]
            for kc in range(8):
                x_ = xs[kc % 2]
                P.dma("sp", x_[:], xT_in.ref(xT_in.h[kc * 128:(kc + 1) * 128, :]))
                P.dma("sp", XT.ref(XT.h[kc * 128:(kc + 1) * 128, :]), x_[:])

        def layer_norm(y, n, l, gc, bc, work):
            sq, mean, rstd = work
            yv = sub(y, y.h[:, :, :n])
            P.act(sub(sq, sq.h[:, :, :n]), yv, AF.Square)
            s1 = sub(ps[0], ps[0].h[:, :n])
            s2 = sub(ps[1], ps[1].h[:, :n])
            P.mm(s1, [(onesf[:], sub(y, y.h[:, m, :n])) for m in range(8)])
            P.mm(s2, [(onesf[:], sub(sq, sq.h[:, m, :n])) for m in range(8)])
            mn = sub(mean, mean.h[:, :n])
            rs = sub(rstd, rstd.h[:, :n])
            P.amul(mn, s1, 1.0 / D)
            P.tt(rs, mn, mn, ALU.mult)
            P.stt(rs, s2, 1.0 / D, rs, ALU.mult, ALU.subtract)
            P.ts(rs, rs, LN_EPS, ALU.add)
            P.act(rs, rs, AF.Sqrt)
            P.recip(rs, rs)
            bc3 = lambda t_, ap2: Ref(ap2.unsqueeze(1).broadcast_to([128, 8, n]), [t_.buf])
            P.tt(yv, yv, bc3(mean, mean.h[:, :n]), ALU.subtract)
            P.tt(yv, yv, bc3(rstd, rstd.h[:, :n]), ALU.mult)
            P.tt(yv, yv, Ref(ppall.h[:, l, gc:gc + 8].unsqueeze(2).broadcast_to([128, 8, n]), [ppall.buf]), ALU.mult)
            P.tt(yv, yv, Ref(ppall.h[:, l, bc:bc + 8].unsqueeze(2).broadcast_to([128, 8, n]), [ppall.buf]), ALU.add)

        def sin_rr(a, tmp):
            P.ts(tmp, a, 1.0 / TWO_PI, ALU.mult, MAGIC, ALU.add)
            P.ts(tmp, tmp, -MAGIC, ALU.add)
            P.stt(a, tmp, -TWO_PI, a, ALU.mult, ALU.add)
            P.act(a, a, AF.Sin)

        cast_rr = [0]

        def cast(out, in_):
            e = ("dve", "act", "dve", "act", "pool", "act", "dve", "act")[cast_rr[0] % 8]
            cast_rr[0] += 1
            P.copy(out, in_, e=e)

        for l in range(L):
            last = (l == L - 1)
            with P.phase():
                win = P.sb("win", [128, 8, DIN], BF16)
                wst = [P.sb("wst%d" % i, [128, 1920], F32) for i in range(2)]
                i = 0
                for kc in range(8):
                    for q in range(4):
                        s_ = wst[i % 2]
                        P.dma("sp", s_[:], w_in.ref(w_in.h[l, kc * 128:(kc + 1) * 128, q * 1920:(q + 1) * 1920]))
                        cast(sub(win, win.h[:, kc, q * 1920:(q + 1) * 1920]), s_[:])
                        i += 1
                xts = [P.sb("xt%d" % i, [128, 8, 512], F32) for i in range(1)]
                hls = [P.sb("hl%d" % i, [128, 8, 512], BF16) for i in range(2)]
                stg = [P.sb("stg%d" % i, [128, 512], F32) for i in range(6)]
                vst = [P.sb("vst%d" % i, [128, 512], BF16) for i in range(2)]
                si = 0
                for ti, (t0, n) in enumerate(TT):
                    kind = 0 if ti < 8 else 1
                    xt = xts[0]
                    hl = hls[ti % 2]
                    for kc in range(8):
                        P.dma("sp", sub(xt, xt.h[:, kc, :n]), XT.ref(XT.h[kc * 128:(kc + 1) * 128, t0:t0 + n]))
                    for kc in range(8):
                        P.act(sub(hl, hl.h[:, kc, :n]), sub(xt, xt.h[:, kc, :n]), AF.Identity,
                              bias=mod(l, 0, kc, kind), scale=mod(l, 1, kc, kind))
                    for j in range(DIN // 128):
                        if 32 <= j < 36:
                            continue
                        pb = ps[j % 6]
                        o = sub(pb, pb.h[:, :n])
                        P.mm(o, [(sub(win, win.h[:, kc, j * 128:(j + 1) * 128]), sub(hl, hl.h[:, kc, :n])) for kc in range(8)])
                        s_ = stg[si % 6]
                        so = sub(s_, s_.h[:, :n])
                        if si % 2 == 0:
                            P.copy(so, o, e="dve")
                        else:
                            P.copy(so, o, e="act")
                        P.dma("sp", PD.ref(PD.h[j * 128:(j + 1) * 128, t0:t0 + n]), so)
                        si += 1
                    for s in range(n // 128):
                        pb = ps[6 + s % 2]
                        P.mm(pb[:], [(sub(hl, hl.h[:, kc, s * 128:(s + 1) * 128]), sub(win, win.h[:, kc, OFF_NA + 1024:OFF_NA + 1536])) for kc in range(8)])
                        v_ = vst[s % 2]
                        P.copy(v_[:], pb[:], e="dve" if s % 2 else "act")
                        P.dma("sp", VT.ref(VT.h[t0 + s * 128:t0 + (s + 1) * 128, :]), v_[:])

            with P.phase():
                uu2 = [[P.sb("u%d%d" % (k, i), [128, T], F32) for i in range(3)] for k in range(2)]
                o3 = [P.sb("o%d" % i, [128, T], F32) for i in range(3)]
                zb = P.sb("zb", [128, T], BF16)
                zts = [P.sb("zts%d" % i, [128, 4, 128], BF16) for i in range(2)]

                def dwconv(o, uu, wcol, bcol, eng):
                    if bcol is None:
                        P.amul(o[:], uu[:], ppc(l, wcol + 1))
                    else:
                        P.act(o[:], uu[:], AF.Identity, bias=ppc(l, bcol), scale=ppc(l, wcol + 1))
                    for (a, Lc) in ((0, NLAT), (NLAT, NCTX)):
                        P.stt(sub(o, o.h[:, a + 1:a + Lc]), sub(uu, uu.h[:, a:a + Lc - 1]), ppc(l, wcol), sub(o, o.h[:, a + 1:a + Lc]), ALU.mult, ALU.add, e=eng)
                        P.stt(sub(o, o.h[:, a:a + Lc - 1]), sub(uu, uu.h[:, a + 1:a + Lc]), ppc(l, wcol + 2), sub(o, o.h[:, a:a + Lc - 1]), ALU.mult, ALU.add, e=eng)

                for j in range(4):
                    u = uu2[j % 2]
                    for q in range(3):
                        r0 = OFF_HY + q * 512 + j * 128
                        P.dma("sp", u[q][:], PD.ref(PD.h[r0:r0 + 128, :]))
                    for q in range(3):
                        c12 = q * 4 + j
                        dwconv(o3[q], u[q], 48 + c12 * 3, 84 + c12, "pool" if q == 0 else "dve")
                    P.tt(u[2][:], o3[2][:], o3[1][:], ALU.mult)
                    P.dma("sp", ZD.ref(ZD.h[j * 128:(j + 1) * 128, :]), u[2][:])
                    P.dma("sp", X0D.ref(X0D.h[j * 128:(j + 1) * 128, :]), o3[0][:])
                    P.copy(zb[:], u[2][:], e="act")
                    nblk = T // 128
                    g0 = 0
                    gi = 0
                    while g0 < nblk:
                        g = min(4, nblk - g0)
                        pb = ps[gi % 2]
                        for b in range(g):
                            P.mm(sub(pb, pb.h[:, b * 128:(b + 1) * 128]), [(sub(zb, zb.h[:, (g0 + b) * 128:(g0 + b + 1) * 128]), identb[:])])
                        z_ = zts[gi % 2]
                        P.copy(sub(z_, z_.h[:, 0:g, :]), sub(pb, pb.h[:, 0:g * 128].rearrange("p (b c) -> p b c", b=g)), e="dve" if gi % 2 else "act")
                        P.dma("sp", ZT.ref(ZT.h[g0 * 128:(g0 + g) * 128, j * 128:(j + 1) * 128].rearrange("(b p) c -> p b c", p=128)),
                              sub(z_, z_.h[:, 0:g, :]))
                        g0 += g
                        gi += 1
                for j in range(4):
                    u = uu2[j % 2]
                    for q in range(3):
                        r0 = OFF_SC + q * 512 + j * 128
                        P.dma("sp", u[q][:], PD.ref(PD.h[r0:r0 + 128, :]))
                    P.tt(u[1][:], u[1][:], u[2][:], ALU.mult, e="pool")
                    dwconv(o3[1], u[1], 100 + j * 3, None, "dve")
                    P.tt(zb[:], u[0][:], o3[1][:], ALU.mult)
                    P.dma("sp", YB[1].ref(YB[1].h[j * 128:(j + 1) * 128, :]), zb[:])

            for (Lc, fe_d, de_d, HT_d, Fc_d, Fs_d, GRE_d, GIM_d) in ((NLAT, featsT, decay, HTD, Fc, Fs, GRE, GIM),
                                                                      (NCTX, cfeatsT, cdecay, cHTD, cFc, cFs, cGRE, cGIM)):
                nb = Lc // 128
                with P.phase():
                    w1s = P.sb("w1s", [33, 64], F32)
                    w2s = P.sb("w2s", [64, 64], F32)
                    w3s = P.sb("w3s", [64, 1024], F32)
                    P.dma("sp", w1s[:], hy_w1[l])
                    P.dma("sp", w2s[:], hy_w2[l])
                    P.dma("sp", w3s[:], hy_w3[l])
                    b1, b2, f1, f2 = (ppc(l, 144 + i, 1, 0, 64) for i in range(4))
                    ft = P.sb("ft", [33, 512], F32)
                    a1 = P.sb("a1", [64, 512], F32)
                    a2 = P.sb("a2", [64, 512], F32)
                    tmp = P.sb("tmp", [64, 512], F32)
                    dec = [P.sb("dec%d" % i, [128, 512], F32) for i in range(2)]
                    hb = [P.sb("hb%d" % i, [128, 1024], BF16) for i in range(2)]
                    hfb = [P.sb("hfb%d" % i, [128, 512], F32) for i in range(2)]
                    n = min(512, Lc)
                    di = 0
                    for ch in range(max(1, Lc // 512)):
                        P.dma("sp", sub(ft, ft.h[:, :n]), fe_d.ref(fe_d.h[:, ch * 512:ch * 512 + n]))
                        p0 = sub(ps[0], ps[0].h[0:64, :n])
                        P.mm(p0, [(w1s[:], sub(ft, ft.h[:, :n]))])
                        a1r = sub(a1, a1.h[:, :n])
                        tr = sub(tmp, tmp.h[:, :n])
                        P.ts(a1r, p0, b1, ALU.add, f1, ALU.mult)
                        sin_rr(a1r, tr)
                        p1 = sub(ps[1], ps[1].h[0:64, :n])
                        P.mm(p1, [(w2s[:], a1r)])
                        a2r = sub(a2, a2.h[:, :n])
                        P.ts(a2r, p1, b2, ALU.add, f2, ALU.mult)
                        sin_rr(a2r, tr)
                        for s in range(n // 128):
                            row0 = ch * 512 + s * 128
                            h_ = hb[s % 2]
                            d_ = dec[di % 2]
                            di += 1
                            P.dma("sp", d_[:], de_d.ref(de_d.h[row0:row0 + 128, :]))
                            for half in range(2):
                                pb = ps[2 + half]
                                P.mm(pb[:], [(sub(a2, a2.h[:, s * 128:(s + 1) * 128]), sub(w3s, w3s.h[:, half * 512:(half + 1) * 512]))])
                                P.tt(hfb[half][:], pb[:], d_[:], ALU.mult)
                            if row0 == 0:
                                P.memset(sub(hfb[1], hfb[1].h[0:1, :]), 0.0, e="dve")
                            P.tt(sub(h_, h_.h[:, 0:512]), hfb[0][:], hfb[1][:], ALU.add)
                            P.tt(sub(h_, h_.h[:, 512:1024]), hfb[1][:], hfb[0][:], ALU.subtract)
                            P.dma("sp", HT_d.ref(HT_d.h[row0:row0 + 128, :]), h_[:])
                with P.phase():
                    hts = P.sb("hts", [128, nb, 1024], BF16)
                    for b0 in range(0, nb, 8):
                        b1_ = min(nb, b0 + 8)
                        P.dma("sp", sub(hts, hts.h[:, b0:b1_, :]), HT_d.ref(HT_d.h[b0 * 128:b1_ * 128, :].rearrange("(b p) c -> p b c", p=128)))
                    fcs = [P.sb("fc%d" % i, [128, nb, 128], BF16) for i in range(4)]
                    fss = [P.sb("fs%d" % i, [128, nb, 128], BF16) for i in range(4)]
                    tc_ = P.sb("tc", [128, 512], F32)
                    ts_ = P.sb("ts", [128, 512], F32)
                    gre = [P.sb("gre%d" % i, [128, 512], F32) for i in range(2)]
                    gim = [P.sb("gim%d" % i, [128, 512], F32) for i in range(2)]
                    for kt in range(nb):
                        fc = fcs[kt % 4]
                        fs = fss[kt % 4]
                        P.dma("sp", fc[:], Fc_d.ref(Fc_d.h[kt].rearrange("p (b k) -> p b k", b=nb)))
                        P.dma("sp", fs[:], Fs_d.ref(Fs_d.h[kt].rearrange("p (b k) -> p b k", b=nb)))
                        pc_ = ps[(kt % 2) * 2]
                        ps_ = ps[(kt % 2) * 2 + 1]
                        P.mm(pc_[:], [(sub(fc, fc.h[:, b, :]), sub(hts, hts.h[:, b, 0:512])) for b in range(nb)])
                        P.mm(ps_[:], [(sub(fs, fs.h[:, b, :]), sub(hts, hts.h[:, b, 512:1024])) for b in range(nb)])
                        P.copy(gre[kt % 2][:], pc_[:], e="act")
                        P.copy(gim[kt % 2][:], ps_[:], e="dve")
                        P.dma("sp", GRE_d.ref(GRE_d.h[kt * 128:(kt + 1) * 128, :]), gre[kt % 2][:])
                        P.dma("sp", GIM_d.ref(GIM_d.h[kt * 128:(kt + 1) * 128, :]), gim[kt % 2][:])

            for (Lc, tok0, Fc_d, Fs_d, FcT_d, FsT_d, GRE_d, GIM_d) in ((NLAT, 0, Fc, Fs, FcT, FsT, GRE, GIM),
                                                                        (NCTX, NLAT, cFc, cFs, cFcT, cFsT, cGRE, cGIM)):
                nb = Lc // 128
                with P.phase():
                    yre = P.sb("yre", [128, nb, 512], BF16)
                    yim = P.sb("yim", [128, nb, 512], BF16)
                    with P.phase():
                        zt = P.sb("zt", [128, nb, 512], BF16)
                        for b0 in range(0, nb, 8):
                            b1_ = min(nb, b0 + 8)
                            P.dma("sp", sub(zt, zt.h[:, b0:b1_, :]), ZT.ref(ZT.h[tok0 + b0 * 128:tok0 + b1_ * 128, :].rearrange("(b p) c -> p b c", p=128)))
                        fcs = [P.sb("fc%d" % i, [128, nb, 128], BF16) for i in range(4)]
                        fss = [P.sb("fs%d" % i, [128, nb, 128], BF16) for i in range(4)]
                        gre = [P.sb("gre%d" % i, [128, 512], F32) for i in range(2)]
                        gim = [P.sb("gim%d" % i, [128, 512], F32) for i in range(2)]
                        t1 = P.sb("t1", [128, 512], F32)
                        t2 = P.sb("t2", [128, 512], F32)
                        t3 = P.sb("t3", [128, 512], F32)
                        t4 = P.sb("t4", [128, 512], F32)
                        for kt in range(nb):
                            fc = fcs[kt % 4]
                            fs = fss[kt % 4]
                            P.dma("sp", fc[:], Fc_d.ref(Fc_d.h[kt].rearrange("p (b k) -> p b k", b=nb)))
                            P.dma("sp", fs[:], Fs_d.ref(Fs_d.h[kt].rearrange("p (b k) -> p b k", b=nb)))
                            P.dma("sp", gre[kt % 2][:], GRE_d.ref(GRE_d.h[kt * 128:(kt + 1) * 128, :]))
                            P.dma("sp", gim[kt % 2][:], GIM_d.ref(GIM_d.h[kt * 128:(kt + 1) * 128, :]))
                            pc = ps[(kt % 2) * 2]
                            pS = ps[(kt % 2) * 2 + 1]
                            P.mm(pc[:], [(sub(fc, fc.h[:, b, :]), sub(zt, zt.h[:, b, :])) for b in range(nb)])
                            P.mm(pS[:], [(sub(fs, fs.h[:, b, :]), sub(zt, zt.h[:, b, :])) for b in range(nb)])
                            gr = gre[kt % 2]
                            gi_ = gim[kt % 2]
                            P.tt(t1[:], pc[:], gr[:], ALU.mult)
                            P.tt(t2[:], pS[:], gi_[:], ALU.mult)
                            P.tt(sub(yre, yre.h[:, kt, :]), t1[:], t2[:], ALU.add, e="pool")
                            P.tt(t3[:], pS[:], gr[:], ALU.mult)
                            P.tt(t4[:], pc[:], gi_[:], ALU.mult)
                            P.tt(sub(yim, yim.h[:, kt, :]), t3[:], t4[:], ALU.subtract, e="pool")
                    with P.phase():
                        TW = 256
                        fct = [P.sb("fct%d" % i, [128, nb, TW], BF16) for i in range(3)]
                        fst = [P.sb("fst%d" % i, [128, nb, TW], BF16) for i in range(3)]
                        zs = [P.sb("zs%d" % i, [128, TW], F32) for i in range(2)]
                        x0s = [P.sb("x0s%d" % i, [128, TW], F32) for i in range(2)]
                        yo = [P.sb("yo%d" % i, [128, TW], BF16) for i in range(2)]
                        tmp = [P.sb("tmpc%d" % i, [128, TW], F32) for i in range(2)]
                        it = 0
                        for tt_ in range(Lc // TW):
                            fc = fct[tt_ % 3]
                            fs = fst[tt_ % 3]
                            P.dma("sp", fc[:], FcT_d.ref(FcT_d.h[tt_].rearrange("p (b t) -> p b t", b=nb)))
                            P.dma("sp", fs[:], FsT_d.ref(FsT_d.h[tt_].rearrange("p (b t) -> p b t", b=nb)))
                            c0 = tok0 + tt_ * TW
                            for cj in range(4):
                                pb = ps[it % 4]
                                o = sub(pb, pb.h[:, :TW])
                                P.mm(o, [(sub(yre, yre.h[:, kb, cj * 128:(cj + 1) * 128]), sub(fc, fc.h[:, kb, :])) for kb in range(nb)]
                                     + [(sub(yim, yim.h[:, kb, cj * 128:(cj + 1) * 128]), sub(fs, fs.h[:, kb, :])) for kb in range(nb)])
                                z_ = zs[it % 2]
                                x_ = x0s[it % 2]
                                P.dma("sp", z_[:], ZD.ref(ZD.h[cj * 128:(cj + 1) * 128, c0:c0 + TW]))
                                P.dma("sp", x_[:], X0D.ref(X0D.h[cj * 128:(cj + 1) * 128, c0:c0 + TW]))
                                t_ = tmp[it % 2]
                                P.ts(t_[:], z_[:], ppc(l, 96 + cj), ALU.mult, e="pool")
                                P.stt(t_[:], o, 1.0 / Lc, t_[:], ALU.mult, ALU.add)
                                P.tt(yo[it % 2][:], t_[:], x_[:], ALU.mult, e="pool")
                                P.dma("sp", YB[0].ref(YB[0].h[cj * 128:(cj + 1) * 128, c0:c0 + TW]), yo[it % 2][:])
                                it += 1

            with P.phase():
                bt = P.sb("bt", [64, 8, 15 * 64], BF16)
                with P.phase():
                    bst = P.sb("bst", [64, 8 * 15 * 64], F32)
                    P.dma("sp", bst[:], rpbx[l])
                    P.amul(sub(bt, bt.h[:].rearrange("p h x -> p (h x)")), bst[:], 8.0)
                vall = P.sb("vall", [64, 64, 512], BF16)
                for r0 in range(0, 64, 16):
                    P.dma("sp", sub(vall, vall.h[:, r0:r0 + 16, :]), VT.ref(VT.h[r0 * 64:(r0 + 16) * 64, :].rearrange("(r p) f -> p r f", p=64)))
                vctx = P.sb("vctx", [128, 2, 512], BF16)
                P.dma("sp", vctx[:], VT.ref(VT.h[NLAT:T, :].rearrange("(b p) f -> p b f", p=128)))
                qst = P.sb("qst", [64, T], F32)
                qTs = [P.sb("qT%d" % i, [64, T], BF16) for i in range(2)]
                kTs = [P.sb("kT%d" % i, [64, T], BF16) for i in range(2)]
                ring = [P.sb("pk%d" % i, [64, 768], BF16) for i in range(32)]
                pctxs = [P.sb("pctx%d" % i, [128, 2, 512], BF16) for i in range(2)]
                rec = P.sb("rec", [64, 512], F32)
                ynas = [P.sb("yna%d" % i, [64, 512], BF16) for i in range(2)]
                id64 = sub(identb, identb.h[0:64, 0:64])
                rlo = lambda kr: 0 if kr <= 7 else kr - 3
                rhi = lambda kr: 63 if kr >= 56 else kr + 4
                rsf = lambda r: min(max(r - 4, 0), 56)
                blk = 0
                sci = 0
                for h in range(8):
                    qT = qTs[h % 2]
                    kT = kTs[h % 2]
                    P.dma("sp", qst[:], PD.ref(PD.h[OFF_NA + h * 64:OFF_NA + (h + 1) * 64, :]))
                    P.copy(qT[:], qst[:], e="dve")
                    P.dma("sp", qst[:], PD.ref(PD.h[OFF_NA + 512 + h * 64:OFF_NA + 512 + (h + 1) * 64, :]))
                    P.copy(kT[:], qst[:], e="act")
                    done_kr = -1
                    for qb in range(9):
                        lat = qb < 8
                        W = 512 if lat else 256
                        q0 = qb * 512
                        if lat:
                            kmin = rsf(8 * qb)
                            kmax = rsf(8 * qb + 7) + 7
                            for kr in range(done_kr + 1, kmax + 1):
                                a, b_ = rlo(kr), rhi(kr)
                                pk = ring[kr % 32]
                                ca = a
                                while ca <= b_:
                                    cb = min(b_, ca + 7)
                                    wN = (cb - ca + 1) * 64
                                    pS = ps[sci % 2]
                                    sci += 1
                                    oap = pS.h[0:64, 0:wN]
                                    P.mmx([(oap, sub(kT, kT.h[:, kr * 64:(kr + 1) * 64]), sub(qT, qT.h[:, ca * 64:(cb + 1) * 64]), True, False),
                                           (oap, id64, sub(bt, bt.h[:, h, (7 - kr + ca) * 64:(7 - kr + cb + 1) * 64]), False, True)],
                                          [sub(pS, oap)])
                                    P.act(sub(pk, pk.h[:, (ca - a) * 64:(cb - a + 1) * 64]), sub(pS, oap), AF.Exp, scale=0.125)
                                    ca = cb + 1
                            done_kr = max(done_kr, kmax)
                        pctx = pctxs[blk % 2]
                        qblk = sub(qT, qT.h[:, q0:q0 + W])
                        for t in range(2):
                            pB = ps[2 + t]
                            P.mmx([(pB.h[:, 0:W], sub(kT, kT.h[:, NLAT + t * 128:NLAT + (t + 1) * 128]), qblk, True, True)], [sub(pB, pB.h[:, 0:W])])
                            P.act(sub(pctx, pctx.h[:, t, 0:W]), sub(pB, pB.h[:, 0:W]), AF.Exp, scale=0.125)
                        pO = ps[4 + blk % 2]
                        pD = ps[6 + blk % 2]
                        loc = []
                        if lat:
                            for kr in range(kmin, kmax + 1):
                                ra = max(rlo(kr), 8 * qb)
                                rb_ = min(rhi(kr), 8 * qb + 7)
                                if ra > rb_:
                                    continue
                                pk = ring[kr % 32]
                                loc.append(((ra - 8 * qb) * 64, (rb_ - 8 * qb + 1) * 64, kr,
                                            sub(pk, pk.h[:, (ra - rlo(kr)) * 64:(rb_ - rlo(kr) + 1) * 64])))
                        for (pX, isden) in ((pO, False), (pD, True)):
                            steps = []
                            c0 = sub(pctx, pctx.h[:, 0, 0:W])
                            c1 = sub(pctx, pctx.h[:, 1, 0:W])
                            l0 = onesb[:] if isden else sub(vctx, vctx.h[:, 0, h * 64:(h + 1) * 64])
                            l1 = onesb[:] if isden else sub(vctx, vctx.h[:, 1, h * 64:(h + 1) * 64])
                            steps.append((pX.h[0:64, 0:W], l0, c0, True, False))
                            for (x0, x1, kr, pr_) in loc:
                                lv = sub(onesb, onesb.h[0:64, :]) if isden else sub(vall, vall.h[:, kr, h * 64:(h + 1) * 64])
                                steps.append((pX.h[0:64, x0:x1], lv, pr_, False, False))
                            steps.append((pX.h[0:64, 0:W], l1, c1, False, True))
                            P.mmx(steps, [sub(pX, pX.h[0:64, 0:W])])
                        P.recip(sub(rec, rec.h[:, :W]), sub(pD, pD.h[0:64, :W]))
                        yn = ynas[blk % 2]
                        P.tt(sub(yn, yn.h[:, :W]), sub(pO, pO.h[0:64, :W]), sub(rec, rec.h[:, :W]), ALU.mult)
                        P.dma("sp", YB[2].ref(YB[2].h[h * 64:(h + 1) * 64, q0:q0 + W]), sub(yn, yn.h[:, :W]))
                        blk += 1

            with P.phase():
                wpr = P.sb("wpr", [128, 3, 4, D], BF16)
                wo = P.sb("wo", [128, 8, D], BF16)
                wst = [P.sb("wst%d" % i, [128, D], F32) for i in range(2)]
                i = 0
                for br in range(3):
                    for kc in range(4):
                        s_ = wst[i % 2]
                        P.dma("sp", s_[:], projs[br].ref(projs[br].h[l, kc * 128:(kc + 1) * 128, :]))
                        cast(sub(wpr, wpr.h[:, br, kc, :]), s_[:])
                        i += 1
                for kc in range(8):
                    s_ = wst[i % 2]
                    P.dma("sp", s_[:], w_o.ref(w_o.h[l, kc * 128:(kc + 1) * 128, :]))
                    cast(sub(wo, wo.h[:, kc, :]), s_[:])
                    i += 1
                ybs = [P.sb("yb%d" % i, [128, 3, 4, 512], BF16) for i in range(2)]
                xt = P.sb("xt", [128, 8, 512], F32)
                mg = P.sb("mg", [128, 8, 512], BF16)
                ya = P.sb("ya", [128, 8, 512], F32)
                sq = P.sb("sq", [128, 8, 512], F32)
                mean = P.sb("mean", [128, 512], F32)
                rstd = P.sb("rstd", [128, 512], F32)
                gts = [P.sb("gt%d" % i, [128, 512], F32) for i in range(4)]
                acc = P.sb("acc", [128, 512], F32)
                tm = [P.sb("tm%d" % i, [128, 512], F32) for i in range(2)]
                gi = 0
                for ti, (t0, n) in enumerate(TT):
                    kind = 0 if ti < 8 else 1
                    yb = ybs[ti % 2]
                    for br in range(3):
                        P.dma("sp", sub(yb, yb.h[:, br, :, :n]), YB[br].ref(YB[br].h[:, t0:t0 + n].rearrange("(kc p) t -> p kc t", p=128)))
                    for kc in range(8):
                        P.dma("sp", sub(xt, xt.h[:, kc, :n]), XT.ref(XT.h[kc * 128:(kc + 1) * 128, t0:t0 + n]))
                    for m in range(8):
                        for br in range(3):
                            pb = ps[2 + br]
                            o = sub(pb, pb.h[:, :n])
                            P.mm(o, [(sub(wpr, wpr.h[:, br, kc, m * 128:(m + 1) * 128]), sub(yb, yb.h[:, br, kc, :n])) for kc in range(4)])
                            g_ = gts[gi % 4]
                            gi += 1
                            gr = sub(g_, g_.h[:, :n])
                            r0 = OFF_GATE + br * D + m * 128
                            P.dma("sp", gr, PD.ref(PD.h[r0:r0 + 128, t0:t0 + n]))
                            P.act(gr, gr, AF.Sigmoid)
                            if br == 0:
                                P.tt(sub(acc, acc.h[:, :n]), o, gr, ALU.mult)
                            elif br == 1:
                                P.tt(sub(tm[0], tm[0].h[:, :n]), o, gr, ALU.mult)
                                P.tt(sub(acc, acc.h[:, :n]), sub(acc, acc.h[:, :n]), sub(tm[0], tm[0].h[:, :n]), ALU.add)
                            else:
                                P.tt(sub(tm[1], tm[1].h[:, :n]), o, gr, ALU.mult)
                                P.tt(sub(mg, mg.h[:, m, :n]), sub(acc, acc.h[:, :n]), sub(tm[1], tm[1].h[:, :n]), ALU.add)
                    for m in range(8):
                        pb = ps[5 + m % 2]
                        o = sub(pb, pb.h[:, :n])
                        P.mm(o, [(sub(wo, wo.h[:, kc, m * 128:(m + 1) * 128]), sub(mg, mg.h[:, kc, :n])) for kc in range(8)])
                        xm = sub(xt, xt.h[:, m, :n])
                        P.amul(xm, xm, ALPHA)
                        P.stt(sub(ya, ya.h[:, m, :n]), o, mod(l, 2, m, kind), xm, ALU.mult, ALU.add)
                    layer_norm(ya, n, l, 112, 120, (sq, mean, rstd))
                    for kc in range(8):
                        P.dma("sp", XT.ref(XT.h[kc * 128:(kc + 1) * 128, t0:t0 + n]), sub(ya, ya.h[:, kc, :n]))
                if "x1" in dbg:
                    pass

            for tiles in ((0, 1, 2), (3, 4, 5), (6, 7, 8)):
                with P.phase():
                    tl = [TT[i] for i in tiles]
                    base = tl[0][0]
                    ntok = sum(n for _, n in tl)
                    hb = P.sb("hb", [128, 8, ntok], BF16)
                    acc = P.sb("acc", [128, 8, ntok], F32)
                    wgt = P.sb("wgt", [64, ntok], F32)
                    with P.phase():
                        wr = P.sb("wr", [128, 8, 64], F32)
                        P.dma("sp", wr[:], moe_router.ref(moe_router.h[l].rearrange("(kc p) e -> p kc e", p=128)))
                        xt = P.sb("xt", [128, 8, 512], F32)
                        hf = P.sb("hf", [128, 8, 512], F32)
                        rt = {n_: P.sb("rt_" + n_, [128, 256], F32) for n_ in ("sc", "sl", "sl2", "slm", "em", "wv", "wg")}
                        r8 = {n_: P.sb("r8_" + n_, [128, 32], F32) for n_ in ("m1", "m2", "gs", "s8", "gm", "pen", "t8")}
                        r1 = {n_: P.sb("r1_" + n_, [128, 4], F32) for n_ in ("ss", "rs")}
                        for (t0, n) in tl:
                            off = t0 - base
                            kind = 0 if t0 < NLAT else 1
                            for kc in range(8):
                                P.dma("sp", sub(xt, xt.h[:, kc, :n]), XT.ref(XT.h[kc * 128:(kc + 1) * 128, t0:t0 + n]))
                            for kc in range(8):
                                P.act(sub(hf, hf.h[:, kc, :n]), sub(xt, xt.h[:, kc, :n]), AF.Identity, bias=mod(l, 3, kc, kind), scale=mod(l, 4, kc, kind))
                                P.copy(sub(hb, hb.h[:, kc, off:off + n]), sub(hf, hf.h[:, kc, :n]), e="dve")
                            ns = n // 128
                            G = ns * 8
                            W = ns * 64
                            pr = sub(ps[7], ps[7].h[:, 0:W])
                            for s in range(ns):
                                P.mm(sub(ps[7], ps[7].h[:, s * 64:(s + 1) * 64]), [(sub(hf, hf.h[:, kc, s * 128:(s + 1) * 128]), sub(wr, wr.h[:, kc, :])) for kc in range(8)])
                            R_ = lambda t_, ap: Ref(ap, [t_.buf])
                            sc, sl, sl2, slm, em, wv, wg = (rt[k] for k in ("sc", "sl", "sl2", "slm", "em", "wv", "wg"))
                            m1, m2, gs, s8, gm, pen, t8 = (r8[k] for k in ("m1", "m2", "gs", "s8", "gm", "pen", "t8"))
                            g3 = lambda t_: R_(t_, t_.h[:, 0:W].rearrange("p (g e) -> p g e", e=8))
                            s3 = lambda t_: R_(t_, t_.h[:, 0:W].rearrange("p (s e) -> p s e", e=64))
                            P.act(R_(sc, sc.h[:, 0:W]), pr, AF.Sigmoid)
                            P.tt(s3(sl), s3(sc), Ref(biasb.h[:, l, :].unsqueeze(1).broadcast_to([128, ns, 64]), [biasb.buf]), ALU.add)
                            P.rmax(R_(m1, m1.h[:, 0:G]), g3(sl))
                            P.tt(g3(sl2), g3(sl), R_(m1, m1.h[:, 0:G].unsqueeze(2).broadcast_to([128, G, 8])), ALU.is_equal)
                            P.stt(R_(sl2, sl2.h[:, 0:W]), R_(sl2, sl2.h[:, 0:W]), -1e9, R_(sl, sl.h[:, 0:W]), ALU.mult, ALU.add)
                            P.rmax(R_(m2, m2.h[:, 0:G]), g3(sl2))
                            P.tt(R_(gs, gs.h[:, 0:G]), R_(m1, m1.h[:, 0:G]), R_(m2, m2.h[:, 0:G]), ALU.add)
                            for s in range(ns):
                                P.top8(R_(s8, s8.h[:, s * 8:(s + 1) * 8]), R_(gs, gs.h[:, s * 8:(s + 1) * 8]))
                            g2 = lambda t_: t_.h[:, 0:G].rearrange("p (s g) -> p s g", g=8)
                            P.tt(R_(gm, g2(gm)), R_(gs, g2(gs)), R_(s8, g2(s8)[:, :, 3:4].broadcast_to([128, ns, 8])), ALU.is_ge)
                            P.ts(R_(pen, pen.h[:, 0:G]), R_(gm, gm.h[:, 0:G]), 1e9, ALU.mult, -1e9, ALU.add)
                            P.tt(g3(slm), g3(sl), R_(pen, pen.h[:, 0:G].unsqueeze(2).broadcast_to([128, G, 8])), ALU.add)
                            for s in range(ns):
                                P.top8(R_(t8, t8.h[:, s * 8:(s + 1) * 8]), R_(slm, slm.h[:, s * 64:(s + 1) * 64]))
                            P.tt(s3(em), s3(slm), R_(t8, g2(t8)[:, :, 7:8].broadcast_to([128, ns, 64])), ALU.is_ge)
                            P.tt(R_(wv, wv.h[:, 0:W]), R_(sc, sc.h[:, 0:W]), R_(em, em.h[:, 0:W]), ALU.mult)
                            ss, rs_ = r1["ss"], r1["rs"]
                            P.rsum(R_(ss, ss.h[:, 0:ns]), s3(wv))
                            P.recip(R_(rs_, rs_.h[:, 0:ns]), R_(ss, ss.h[:, 0:ns]))
                            P.stt(s3(wg), s3(wv), 2.5, R_(rs_, rs_.h[:, 0:ns].unsqueeze(2).broadcast_to([128, ns, 64])), ALU.mult, ALU.mult)
                            for s in range(ns):
                                P.mm(sub(ps[6], ps[6].h[0:64, s * 128:(s + 1) * 128]), [(R_(wg, wg.h[:, s * 64:(s + 1) * 64]), identf[:])])
                            P.copy(sub(wgt, wgt.h[:, off:off + n]), sub(ps[6], ps[6].h[0:64, 0:n]), e="act")
                        P.dma("sp", WG.ref(WG.h[:, base:base + ntok]), wgt[:])
                    with P.phase():
                        w13s = P.sb("w13s", [128, 8, 512], F32)
                        w2st = P.sb("w2st", [128, 2, D], F32)
                        w13 = [[P.sb("w13_%d%d" % (i, j), [128, 8, 512], BF16) for j in range(2)] for i in range(2)]
                        w2b = [[P.sb("w2b_%d%d" % (i, j), [128, 2, D], BF16) for j in range(2)] for i in range(2)]
                        s1 = [P.sb("s1_%d" % i, [128, 512], F32) for i in range(2)]
                        t_ = [P.sb("t_%d" % i, [128, 512], F32) for i in range(2)]
                        hh = [[P.sb("hh%d%d" % (i, j), [128, 2, 512], BF16) for j in range(2)] for i in range(2)]
                        wsel = [P.sb("wsel%d" % i, [64, 512], F32) for i in range(2)]
                        wshi = [P.sb("wshi%d" % i, [64, 512], BF16) for i in range(2)]
                        wslo = [P.sb("wslo%d" % i, [64, 512], BF16) for i in range(2)]
                        po_i = [0]

                        gsb = [[P.sb("gsb%d%d" % (i, j), [128, 512], F32) for j in range(2)] for i in range(2)]

                        def GprepA(e, n, off, par, j):
                            if e < 0:
                                return
                            g_ = gsb[par][j]
                            P.dma("sp", sub(g_, g_.h[:, :n]), Ref(WG.h[e:e + 1, base + off:base + off + n].broadcast_to([128, n]), [], True))

                        def GprepB(e, n, off, par, j):
                            return

                        def S1(e, a13, n, off, hcur, j, par):
                            pG = sub(gsb[par][j], gsb[par][j].h[:, :n])
                            for fc in range(2):
                                p1 = sub(ps[fc * 2], ps[fc * 2].h[:, :n])
                                p3 = sub(ps[fc * 2 + 1], ps[fc * 2 + 1].h[:, :n])
                                P.mm(p1, [(sub(a13, a13.h[:, kc, fc * 128:(fc + 1) * 128]), sub(hb, hb.h[:, kc, off:off + n])) for kc in range(8)])
                                P.mm(p3, [(sub(a13, a13.h[:, kc, 256 + fc * 128:256 + (fc + 1) * 128]), sub(hb, hb.h[:, kc, off:off + n])) for kc in range(8)])
                                s_ = sub(s1[fc], s1[fc].h[:, :n])
                                P.act(s_, p1, AF.Silu)
                                if e >= 0:
                                    tr = sub(t_[fc], t_[fc].h[:, :n])
                                    P.tt(tr, p3, s_, ALU.mult)
                                    P.tt(sub(hcur, hcur.h[:, fc, :n]), tr, pG, ALU.mult)
                                else:
                                    P.tt(sub(hcur, hcur.h[:, fc, :n]), p3, s_, ALU.mult)

                        def S2(grp, a2s, n, off, hcs):
                            for m in range(8):
                                pO = ps[4 + po_i[0] % 4]
                                po_i[0] += 1
                                o = sub(pO, pO.h[:, :n])
                                P.mm(o, [(sub(a2s[j], a2s[j].h[:, fc, m * 128:(m + 1) * 128]), sub(hcs[j], hcs[j].h[:, fc, :n]))
                                         for j in range(len(grp)) for fc in range(2)])
                                ar = sub(acc, acc.h[:, m, off:off + n])
                                if grp[0] < 0:
                                    P.copy(ar, o, e="act")
                                else:
                                    P.tt(ar, ar, o, ALU.add)

                        groups = [[-1]] + [[2 * i, 2 * i + 1] for i in range(32)]
                        prev = None
                        it = 0
                        def wload(gi, j):
                            e = groups[gi][j]
                            a13 = w13[gi % 2][j]
                            a2 = w2b[gi % 2][j]
                            if e < 0:
                                srcs = (sh_w1.h[l], sh_w3.h[l], sh_w2.h[l])
                            else:
                                srcs = (moe_w1.h[l, e], moe_w3.h[l, e], moe_w2.h[l, e])
                            P.dma("sp", sub(w13s, w13s.h[:, :, 0:256]), Ref(srcs[0].rearrange("(kc p) f -> p kc f", p=128), [], True))
                            P.dma("sp", sub(w13s, w13s.h[:, :, 256:512]), Ref(srcs[1].rearrange("(kc p) f -> p kc f", p=128), [], True))
                            P.dma("sp", w2st[:], Ref(srcs[2].rearrange("(fc p) m -> p fc m", p=128), [], True))
                            P.copy(a13[:], w13s[:], e="act")
                            P.copy(a2[:], w2st[:], e="act")

                        wload(0, 0)
                        items = [(gi, grp, ti_, t0, n) for gi, grp in enumerate(groups) for ti_, (t0, n) in enumerate(tl)]
                        for k, (gi, grp, ti_, t0, n) in enumerate(items):
                            off = t0 - base
                            hcs = [hh[k % 2][j] for j in range(len(grp))]
                            nxt = items[k + 1] if k + 1 < len(items) else None
                            if nxt is not None:
                                for j, e in enumerate(nxt[1]):
                                    GprepA(e, nxt[4], nxt[3] - base, (k + 1) % 2, j)
                            for j, e in enumerate(grp):
                                S1(e, w13[gi % 2][j], n, off, hcs[j], j, k % 2)
                            if prev is not None:
                                S2(*prev)
                            if nxt is not None:
                                for j, e in enumerate(nxt[1]):
                                    GprepB(e, nxt[4], nxt[3] - base, (k + 1) % 2, j)
                            prev = (grp, [w2b[gi % 2][j] for j in range(len(grp))], n, off, hcs)
                            if gi + 1 < len(groups) and ti_ < len(groups[gi + 1]):
                                wload(gi + 1, ti_)
                        S2(*prev)
                    with P.phase():
                        xt = P.sb("xt", [128, 8, 512], F32)
                        ya = P.sb("ya", [128, 8, 512], F32)
                        mean = P.sb("mean", [128, 512], F32)
                        rstd = P.sb("rstd", [128, 512], F32)
                        for (t0, n) in tl:
                            off = t0 - base
                            kind = 0 if t0 < NLAT else 1
                            for kc in range(8):
                                P.dma("sp", sub(xt, xt.h[:, kc, :n]), XT.ref(XT.h[kc * 128:(kc + 1) * 128, t0:t0 + n]))
                            xv = sub(xt, xt.h[:, :, :n])
                            yv = sub(ya, ya.h[:, :, :n])
                            P.amul(xv, xv, ALPHA)
                            P.tt(yv, sub(acc, acc.h[:, :, off:off + n]),
                                 Ref(mods.h[:, l, 40:48, kind].unsqueeze(2).broadcast_to([128, 8, n]), [mods.buf]), ALU.mult)
                            P.tt(yv, yv, xv, ALU.add)
                            layer_norm(ya, n, l, 128, 136, (xt, mean, rstd))
                            dst = yT if (last and kind == 0) else XT
                            for kc in range(8):
                                P.dma("sp", dst.ref(dst.h[kc * 128:(kc + 1) * 128, t0:t0 + n]), sub(ya, ya.h[:, kc, :n]))

        if "xt" in dbg:
            xdbg = P.dram("xdbg", [D, T], F32, kind="ExternalOutput")
            with P.phase():
                xs = [P.sb("xfin%d" % i, [128, T], F32) for i in range(2)]
                for kc in range(8):
                    x_ = xs[kc % 2]
                    P.dma("sp", x_[:], XT.ref(XT.h[kc * 128:(kc + 1) * 128, :]))
                    P.dma("sp", xdbg.ref(xdbg.h[kc * 128:(kc + 1) * 128, :]), x_[:])
        P.barrier()
        print("build: ninst", P.ninst, "nwait", P.nwait, "nsem", len(P.allsems) + 4)
    return nc


_CONST = {}


def _consts():
    if _CONST:
        return _CONST
    bf = ml_dtypes.bfloat16

    def dft(Lc, TW):
        N = 2 * Lc
        t = np.arange(Lc, dtype=np.int64)
        k = np.arange(Lc, dtype=np.int64)
        j = (t[:, None] * (2 * k[None, :] + 1)) % (2 * N)
        th = j.astype(np.float64) * (2.0 * np.pi / (2 * N))
        c = np.cos(th)
        s = np.sin(th)
        nb = Lc // 128

        def fwd(m):
            return np.ascontiguousarray(m.reshape(nb, 128, nb, 128).transpose(2, 1, 0, 3).reshape(nb, 128, nb * 128)).astype(bf)

        def inv(m):
            mt = m.T
            ntt = Lc // TW
            return np.ascontiguousarray(mt.reshape(nb, 128, ntt, TW).transpose(2, 1, 0, 3).reshape(ntt, 128, nb * TW)).astype(bf)
        return fwd(c), fwd(s), inv(c), inv(s)

    def feats(Lc):
        f32 = np.float32
        t = np.linspace(0.0, 1.0, Lc, dtype=f32)[:, None]
        ang = (f32(2.0 * math.pi / Lc)) * np.arange(Lc, dtype=f32)[:, None]
        bands = np.linspace(1e-4, 15, 16, dtype=f32)[None, :]
        fe = np.concatenate([t, np.cos(bands * ang), -np.sin(bands * ang)], -1).astype(f32)
        deltas = np.abs(np.linspace(math.log(1e-2) / 1.5, math.log(1e-2) / 0.3, 512, dtype=f32))
        dec = np.exp(-t * deltas[None, :]).astype(f32)
        return np.ascontiguousarray(fe.T), dec

    _CONST["Fc"], _CONST["Fs"], _CONST["FcT"], _CONST["FsT"] = dft(NLAT, 256)
    _CONST["cFc"], _CONST["cFs"], _CONST["cFcT"], _CONST["cFsT"] = dft(NCTX, 256)
    _CONST["featsT"], _CONST["decay"] = feats(NLAT)
    _CONST["cfeatsT"], _CONST["cdecay"] = feats(NCTX)
    _CONST["ident"] = np.eye(128, dtype=np.float32)
    return _CONST


def _rpb_expand(na_rpb):
    Lr = na_rpb.shape[0]
    c = np.arange(64)
    cs = np.clip(c - 8, 0, 48)
    kc = np.arange(64)
    inwin = (kc[:, None] >= cs[None, :]) & (kc[:, None] < cs[None, :] + 16)
    rel = np.clip(kc[:, None] - c[None, :] + 15, 0, 30)
    g = na_rpb[:, :, :, rel]
    g = np.where(inwin[None, None, None], g, np.float32(-1e30)).astype(np.float32)
    g = g[:, :, ::-1]
    return np.ascontiguousarray(g.transpose(0, 3, 1, 2, 4)).reshape(Lr, 64, 8 * 15 * 64)


def _pack_pp(inp, nl):
    pp = np.zeros((nl, 128, NPP), np.float32)

    def chunks(v):
        return v.reshape(-1, 128).T
    for l in range(nl):
        pp[l, :, 0:48] = chunks(inp["b_ada"][l])
        cw = inp["hy_conv_w"][l]
        for c12 in range(12):
            for tap in range(3):
                pp[l, :, 48 + c12 * 3 + tap] = cw[tap, c12 * 128:(c12 + 1) * 128]
        pp[l, :, 84:96] = chunks(inp["hy_conv_b"][l])
        pp[l, :, 96:100] = chunks(inp["hy_bias_d"][l])
        sw = inp["sc_conv_w"][l]
        for j in range(4):
            for tap in range(3):
                pp[l, :, 100 + j * 3 + tap] = sw[tap, j * 128:(j + 1) * 128]
        pp[l, :, 112:120] = chunks(inp["ln1_g"][l])
        pp[l, :, 120:128] = chunks(inp["ln1_b"][l])
        pp[l, :, 128:136] = chunks(inp["ln2_g"][l])
        pp[l, :, 136:144] = chunks(inp["ln2_b"][l])
        pp[l, 0:64, 144] = inp["hy_b1"][l]
        pp[l, 0:64, 145] = inp["hy_b2"][l]
        pp[l, 0:64, 146] = inp["hy_sin_freq"][l, 0]
        pp[l, 0:64, 147] = inp["hy_sin_freq"][l, 1]
    return pp


def make_in_maps(inp, cores, nl=DEPTH):
    cst = _consts()
    f = lambda a: np.ascontiguousarray(np.asarray(a, dtype=np.float32))
    shared = {
        "w_ada": f(inp["w_ada"][:nl]), "pp": _pack_pp(inp, nl), "w_in": f(inp["w_in"][:nl]),
        "hy_w1": f(inp["hy_w1"][:nl]), "hy_w2": f(inp["hy_w2"][:nl]), "hy_w3": f(inp["hy_w3"][:nl]),
        "hy_proj": f(inp["hy_proj"][:nl]), "sc_proj": f(inp["sc_proj"][:nl]), "na_proj": f(inp["na_proj"][:nl]),
        "w_o": f(inp["w_o"][:nl]), "rpbx": _rpb_expand(np.asarray(inp["na_rpb"][:nl], np.float32)),
        "moe_router": f(inp["moe_router"][:nl]),
        "moe_biasb": np.ascontiguousarray(np.broadcast_to(np.asarray(inp["moe_bias"][:nl], np.float32)[:, None, :], (nl, 128, 64))),
        "moe_w1": f(inp["moe_w1"][:nl]), "moe_w3": f(inp["moe_w3"][:nl]), "moe_w2": f(inp["moe_w2"][:nl]),
        "sh_w1": f(inp["sh_w1"][:nl]), "sh_w3": f(inp["sh_w3"][:nl]), "sh_w2": f(inp["sh_w2"][:nl]),
    }
    shared.update(cst)
    cc = np.asarray(inp["c_ctx"], np.float32).reshape(8, 128).T
    maps = []
    for b in cores:
        m = dict(shared)
        m["xT"] = np.ascontiguousarray(np.concatenate([np.asarray(inp["x"][b], np.float32).T, np.asarray(inp["ctx"][b], np.float32).T], axis=1))
        cb = np.asarray(inp["c"][b], np.float32).reshape(8, 128).T
        m["cvec"] = np.ascontiguousarray(np.stack([cb, cc], axis=-1))
        maps.append(m)
    return maps


_NC = {}


def kernel(**inputs):
    inp = {k: np.asarray(v) for k, v in inputs.items()}
    if DEPTH not in _NC:
        _NC[DEPTH] = build(DEPTH)
    nc = _NC[DEPTH]
    maps = make_in_maps(inp, list(range(8)))
    res = run_bass_kernel_spmd(nc, maps, core_ids=list(range(8)))
    out = np.stack([np.ascontiguousarray(res.results[b]["yT"].T) for b in range(8)], axis=0)
    return out.astype(np.float32)
```
